# Optimizing a Trainium2 kernel written in Bass

```python
import jax, jax.numpy as jnp
from jax import lax
import numpy as np

D_MODEL = 1024
BATCH = 8
SEQ = 2048
DEPTH = 2

MIX_WIDTH = D_MODEL
RET_WIDTH = MIX_WIDTH // 2
HG_WIDTH = MIX_WIDTH - RET_WIDTH
RET_HEADS = 4
RET_V_DIM = RET_WIDTH // RET_HEADS
RET_QK_DIM = RET_V_DIM // 2
RET_QK_WIDTH = RET_HEADS * RET_QK_DIM
HG_HEADS = 4
HG_DIM = HG_WIDTH // HG_HEADS
CHUNK = 64
D_FF = -(-8 * D_MODEL // (3 * 256)) * 256
ROPE_BASE = 10000.0
EPS = 1e-6
N_MOD = 6
IN_SPLITS = (RET_QK_WIDTH, RET_QK_WIDTH, RET_WIDTH, RET_WIDTH, HG_WIDTH, HG_WIDTH, HG_WIDTH, HG_WIDTH)
IN_WIDTH = sum(IN_SPLITS)
IN_OFFSETS = tuple(int(v) for v in np.cumsum(IN_SPLITS)[:-1])

kernel_name = "hybrid_retention_hgrn2_adaln_block"


def rmsnorm(x, w):
    xf = x.astype(jnp.float32)
    y = xf * lax.rsqrt(jnp.mean(xf * xf, axis=-1, keepdims=True) + EPS)
    return (y * w.astype(jnp.float32)).astype(x.dtype)


def head_rmsnorm(o, w):
    B, S, H, d = o.shape
    y = o * lax.rsqrt(jnp.mean(o * o, axis=-1, keepdims=True) + EPS)
    return y.reshape(B, S, H * d) * w.astype(jnp.float32)


def rotary(x, pos):
    d = x.shape[-1]
    inv = ROPE_BASE ** (-jnp.linspace(0.0, 1.0, d // 2, dtype=jnp.float32))
    theta = pos[:, None] * inv[None, :]
    cos = jnp.cos(theta)[None, :, None, :]
    sin = jnp.sin(theta)[None, :, None, :]
    x1, x2 = x[..., 0::2], x[..., 1::2]
    out = jnp.stack([x1 * cos - x2 * sin, x1 * sin + x2 * cos], axis=-1)
    return out.reshape(x.shape)


def to_chunks(x):
    B, S, H, d = x.shape
    return x.reshape(B, S // CHUNK, CHUNK, H, d).transpose(1, 0, 3, 2, 4)


def from_chunks(y):
    nC, B, H, C, d = y.shape
    return y.transpose(1, 0, 3, 2, 4).reshape(B, nC * C, H, d)


def retention_chunkwise(q, k, v):
    B, S, H, dk = q.shape
    dv = v.shape[-1]
    log_gamma = jnp.log(1.0 - jnp.power(2.0, -5.0 - jnp.arange(H, dtype=jnp.float32)))
    idx = jnp.arange(CHUNK, dtype=jnp.float32)
    rel = idx[:, None] - idx[None, :]
    inner_decay = jnp.exp(jnp.where(rel[None] >= 0, log_gamma[:, None, None] * rel[None], -jnp.inf))
    cross_decay = jnp.exp(log_gamma[:, None] * (idx[None, :] + 1.0))
    state_decay = jnp.exp(log_gamma[:, None] * (CHUNK - 1.0 - idx[None, :]))
    chunk_decay = jnp.exp(log_gamma * CHUNK)

    def step(R, inp):
        qc, kc, vc = inp
        scores = jnp.einsum('bhtd,bhsd->bhts', qc, kc) * inner_decay[None]
        o = (jnp.einsum('bhts,bhsv->bhtv', scores, vc)
             + jnp.einsum('bhtd,bhdv->bhtv', qc, R) * cross_decay[None, :, :, None])
        R = (chunk_decay[None, :, None, None] * R
             + jnp.einsum('bhsd,bhsv->bhdv', kc * state_decay[None, :, :, None], vc))
        return R, o

    R0 = jnp.zeros((B, H, dk, dv), jnp.float32)
    _, o = lax.scan(step, R0, (to_chunks(q), to_chunks(k), to_chunks(v)))
    return from_chunks(o)


def hgrn2_chunkwise(q, k, log_f, v):
    B, S, H, dk = q.shape
    dv = v.shape[-1]
    causal = jnp.tril(jnp.ones((CHUNK, CHUNK), dtype=bool))

    def step(St, inp):
        qc, kc, gc, vc = inp
        b = jnp.cumsum(gc, axis=2)
        diff = b[:, :, :, None, :] - b[:, :, None, :, :]
        decay = jnp.exp(jnp.where(causal[None, None, :, :, None], diff, -jnp.inf))
        scores = jnp.einsum('bhtsd,bhsd->bhts', decay * qc[:, :, :, None, :], kc)
        o = (jnp.einsum('bhts,bhsv->bhtv', scores, vc)
             + jnp.einsum('bhtd,bhdv->bhtv', qc * jnp.exp(b), St))
        b_last = b[:, :, -1:, :]
        St = (jnp.exp(b_last[:, :, 0, :])[..., None] * St
              + jnp.einsum('bhsd,bhsv->bhdv', kc * jnp.exp(b_last - b), vc))
        return St, o

    S0 = jnp.zeros((B, H, dk, dv), jnp.float32)
    _, o = lax.scan(step, S0, (to_chunks(q), to_chunks(k), to_chunks(log_f), to_chunks(v)))
    return from_chunks(o)


def hybrid_mixer(h, layer, w_in, ret_norm_w, hg_lower_bounds, hg_norm_w, w_out):
    B, S, _ = h.shape
    pos = jnp.arange(S, dtype=jnp.float32)
    proj = h @ w_in
    rq, rk, rv, rg, hq, hf, hi, hg = jnp.split(proj, IN_OFFSETS, axis=-1)

    rq = rotary(rq.astype(jnp.float32).reshape(B, S, RET_HEADS, RET_QK_DIM), pos)
    rk = rotary(rk.astype(jnp.float32).reshape(B, S, RET_HEADS, RET_QK_DIM), pos) * (RET_QK_DIM ** -0.5)
    rv = rv.astype(jnp.float32).reshape(B, S, RET_HEADS, RET_V_DIM)
    o_ret = retention_chunkwise(rq, rk, rv)
    o_ret = head_rmsnorm(o_ret, ret_norm_w) * jax.nn.silu(rg.astype(jnp.float32))

    hf = hf.astype(jnp.float32)
    if layer == 0:
        log_f = jax.nn.log_sigmoid(hf)
        k_in = jax.nn.sigmoid(-hf)
    else:
        probs = jax.nn.softmax(hg_lower_bounds.astype(jnp.float32), axis=0)
        lb = (jnp.cumsum(probs, axis=0) - probs[0:1])[layer]
        f = lb + (1.0 - lb) * jax.nn.sigmoid(hf)
        log_f = jnp.log(f)
        k_in = 1.0 - f
    hq = jax.nn.silu(hq.astype(jnp.float32)).reshape(B, S, HG_HEADS, HG_DIM)
    o_hg = hgrn2_chunkwise(hq,
                           k_in.reshape(B, S, HG_HEADS, HG_DIM),
                           log_f.reshape(B, S, HG_HEADS, HG_DIM),
                           hi.astype(jnp.float32).reshape(B, S, HG_HEADS, HG_DIM))
    o_hg = head_rmsnorm(o_hg, hg_norm_w) * jax.nn.silu(hg.astype(jnp.float32))

    o = jnp.concatenate([o_ret, o_hg], axis=-1).astype(h.dtype)
    return o @ w_out


def swiglu(h, w_gate, w_up, w_down):
    return (jax.nn.silu(h @ w_gate) * (h @ w_up)) @ w_down


def setup_inputs(seed: int = 0) -> dict:
    key = jax.random.key(seed)
    ks = jax.random.split(key, 16)
    f32 = jnp.float32
    nrm = lambda k, shape, s: jax.random.normal(k, shape, f32) * s
    return {
        "x": nrm(ks[0], (BATCH, SEQ, D_MODEL), 1.0),
        "c": nrm(ks[1], (BATCH, D_MODEL), 1.0),
        "w_ada": nrm(ks[2], (DEPTH, D_MODEL, N_MOD * D_MODEL), 0.5 * D_MODEL ** -0.5),
        "b_ada": nrm(ks[3], (DEPTH, N_MOD * D_MODEL), 0.01),
        "norm_mix_w": 1.0 + nrm(ks[4], (DEPTH, D_MODEL), 0.02),
        "w_in": nrm(ks[5], (DEPTH, D_MODEL, IN_WIDTH), D_MODEL ** -0.5),
        "ret_norm_w": 1.0 + nrm(ks[6], (DEPTH, RET_WIDTH), 0.02),
        "hg_lower_bounds": nrm(ks[7], (DEPTH, HG_WIDTH), 0.5),
        "hg_norm_w": 1.0 + nrm(ks[8], (DEPTH, HG_WIDTH), 0.02),
        "w_out": nrm(ks[9], (DEPTH, MIX_WIDTH, D_MODEL), MIX_WIDTH ** -0.5),
        "norm_ffn_w": 1.0 + nrm(ks[10], (DEPTH, D_MODEL), 0.02),
        "w_ffn_gate": nrm(ks[11], (DEPTH, D_MODEL, D_FF), D_MODEL ** -0.5),
        "w_ffn_up": nrm(ks[12], (DEPTH, D_MODEL, D_FF), D_MODEL ** -0.5),
        "w_ffn_down": nrm(ks[13], (DEPTH, D_FF, D_MODEL), D_FF ** -0.5),
        "final_norm_w": 1.0 + nrm(ks[14], (D_MODEL,), 0.02),
    }


def reference(x, c, w_ada, b_ada, norm_mix_w, w_in, ret_norm_w, hg_lower_bounds, hg_norm_w, w_out,
              norm_ffn_w, w_ffn_gate, w_ffn_up, w_ffn_down, final_norm_w):
    c_act = jax.nn.silu(c)
    for layer in range(DEPTH):
        mod = c_act @ w_ada[layer] + b_ada[layer]
        sh1, sc1, g1, sh2, sc2, g2 = jnp.split(mod[:, None, :], N_MOD, axis=-1)
        h = rmsnorm(x, norm_mix_w[layer]) * (1.0 + sc1) + sh1
        x = x + g1 * hybrid_mixer(h, layer, w_in[layer], ret_norm_w[layer], hg_lower_bounds,
                                  hg_norm_w[layer], w_out[layer])
        h = rmsnorm(x, norm_ffn_w[layer]) * (1.0 + sc2) + sh2
        x = x + g2 * swiglu(h, w_ffn_gate[layer], w_ffn_up[layer], w_ffn_down[layer])
    return rmsnorm(x, final_norm_w)
```

```python
import numpy as np
import concourse.bass as bass
import concourse.mybir as mybir
from concourse.bass_utils import run_bass_kernel_spmd

F32 = mybir.dt.float32
BF16 = mybir.dt.bfloat16
AF = mybir.ActivationFunctionType
ALU = mybir.AluOpType

D = 1024
T = 2048
NT = 16
KC = 8
DFF = 2816
NJ = 22
L = 2
EPS = 1e-6
DEBUG_MEM = False
SAME_ENGINE_SYNC = True

C_NW1 = 0
C_NW2 = 8
C_RNW = 16
C_HNW = 20
C_LB = 48
C_C = 56
NCOLS = 64


def I(name, *args, **kwargs):
    return (name, args, kwargs)


class Sem:
    def __init__(self, handle, name):
        self.h = handle
        self.name = name
        self.count = 0


class Ev:
    __slots__ = ("sem", "val", "clock")

    def __init__(self, sem, val, clock):
        self.sem = sem
        self.val = val
        self.clock = clock


class Tok:
    __slots__ = ("name", "lw", "rd")

    def __init__(self, name=""):
        self.name = name
        self.lw = None
        self.rd = []


class Sched:
    ENGS = ("pe", "act", "dve", "pool", "sp")

    def __init__(self, nc):
        self.nc = nc
        self.sem = {e: Sem(nc.alloc_semaphore("cnt_" + e), "cnt_" + e) for e in self.ENGS}
        self.known = {e: {} for e in self.ENGS}
        self.prog = {e: [] for e in self.ENGS}
        self.free_dma_sems = {}
        self.n_dma_sems = 0

    def dma_sem(self, q="pool"):
        fl = self.free_dma_sems.setdefault(q, [])
        if fl:
            return fl.pop()
        self.n_dma_sems += 1
        name = "dma%s%d" % (q, self.n_dma_sems)
        s = Sem(self.nc.alloc_semaphore(name), name)
        s.q = q
        return s

    def release_dma_sem(self, s):
        self.free_dma_sems[s.q].append(s)

    def emit(self, eng, fn, reads=(), writes=(), dsem=None):
        evs = []
        for t in reads:
            if t.lw is not None:
                evs.append(t.lw)
        for t in writes:
            if t.lw is not None:
                evs.append(t.lw)
            evs.extend(t.rd)
        known = self.known[eng]
        own = self.sem[eng]
        need = {}
        for ev in evs:
            if ev.sem is own and (eng == "pe" or not SAME_ENGINE_SYNC):
                continue
            if known.get(ev.sem.name, 0) >= ev.val:
                continue
            cur = need.get(ev.sem.name)
            if cur is None or cur.val < ev.val:
                need[ev.sem.name] = ev
        waits = []
        for name, ev in need.items():
            waits.append((ev.sem.h, ev.val))
            for k, v in ev.clock.items():
                if known.get(k, 0) < v:
                    known[k] = v
            if known.get(name, 0) < ev.val:
                known[name] = ev.val
        if fn is None:
            self.prog[eng].append((waits, None, None))
            return None
        if dsem is not None:
            dsem.count += 16
            clock = dict(known)
            clock[own.name] = own.count
            ev = Ev(dsem, dsem.count, clock)
            inc = (dsem.h, 16)
        else:
            own.count += 1
            clock = dict(known)
            clock[own.name] = own.count
            ev = Ev(own, own.count, clock)
            inc = (own.h, 1)
        for t in writes:
            t.lw = ev
            t.rd = []
        for t in reads:
            t.rd.append(ev)
        self.prog[eng].append((waits, fn, inc))
        return ev

    def barrier(self):
        evs = {}
        for e in self.ENGS:
            s = self.sem[e]
            if s.count > 0:
                clock = dict(self.known[e])
                clock[s.name] = s.count
                evs[e] = Ev(s, s.count, clock)
        for e in self.ENGS:
            t = Tok()
            for e2, ev in evs.items():
                if e2 == e:
                    continue
                t.lw = ev
                self.emit(e, None, reads=(t,))

    def replay(self, eng, engine_obj):
        for waits, fn, inc in self.prog[eng]:
            for (h, v) in waits:
                engine_obj.wait_ge(h, v)
            if fn is not None:
                ins = getattr(engine_obj, fn[0])(*fn[1], **fn[2])
                ins.then_inc(inc[0], inc[1])


def build_nc(n_stage=99):
    nc = bass.Bass("TRN2", target_bir_lowering=False)
    S = Sched(nc)

    def dram(name, shape, kind="ExternalInput"):
        return nc.dram_tensor(name, list(shape), F32, kind=kind).ap()

    x_d = dram("x", [T, D])
    cols_d = dram("cols", [128, NCOLS])
    consts_d = dram("consts", [128, 128 * 3 + 512])
    fw_d = dram("fwrow", [128, D])
    rot_d = dram("rot", [4, 128, 4, 2, 512])
    wada_d = dram("w_ada", [L, 12, 128, 9, 512])
    wrqk_d = dram("w_rqk", [L, 128, 8, 1024])
    wrv_d = dram("w_rv", [L, 128, 8, 512])
    wrg_d = dram("w_rg", [L, 128, 8, 512])
    whq_d = dram("w_hq", [L, 128, 8, 512])
    whf_d = dram("w_hf", [L, 128, 8, 512])
    whi_d = dram("w_hi", [L, 128, 8, 512])
    whg_d = dram("w_hg", [L, 128, 8, 512])
    wo_d = dram("w_o", [L, 128, 8, 1024])
    wgs_d = dram("w_gs", [L, NJ, 128, 8, 128])
    wus_d = dram("w_us", [L, NJ, 128, 8, 128])
    wd_d = dram("w_d", [L, 128, NJ, 1024])
    out_d = dram("out", [T, D], kind="ExternalOutput")

    cnt = [0]

    def sb(shape, dt=F32, name=None):
        cnt[0] += 1
        return nc.alloc_sbuf_tensor("%s_%d" % (name or "sb", cnt[0]), list(shape), dt)

    def ps(dt=F32, name=None):
        cnt[0] += 1
        shape = [128, 512] if dt == F32 else [128, 1024]
        return nc.alloc_psum_tensor("%s_%d" % (name or "ps", cnt[0]), shape, dt)

    def snap():
        return (nc.sbuf_base, nc.sbuf_top, nc.psum_base, nc.psum_top)

    def restore(s):
        if DEBUG_MEM:
            print("sbuf bytes remaining at scope end:", nc.sbuf_bytes_remaining)
        nc.sbuf_base, nc.sbuf_top, nc.psum_base, nc.psum_top = s

    x_sb = sb([128, NT, D], F32, "x")
    x_tok = [Tok("x%d" % i) for i in range(NT)]
    cols = sb([128, NCOLS], F32, "cols")
    consts = sb([128, 128 * 2 + 512], F32, "consts")
    ident = sb([128, 128], BF16, "ident")
    ones_bf = sb([128, 128], BF16, "ones_bf")
    ones_f = sb([128, 128], F32, "ones_f")
    grow = sb([128, 2, D], F32, "grow")
    modcols = sb([128, 32], F32, "modcols")
    wsc = sb([128, 16], F32, "wsc")
    lbc = sb([128, 24], F32, "lbc")
    c_act = sb([128, 8], F32, "c_act")
    c_rep = sb([128, 9, 128], BF16, "c_rep")
    ssq = sb([128, NT], F32, "ssq")
    rstdx = sb([128, NT], F32, "rstdx")
    mask_ret = consts[:, 0:128]
    mask_hg = consts[:, 128:256]
    resetmask = consts[:, 256:768]

    t_const = Tok("const")
    t_grow = [Tok("g1"), Tok("g2")]
    t_modcols = Tok("modcols")
    t_wsc = Tok("wsc")
    t_crep = Tok("crep")
    t_ssq = Tok("ssq")
    t_rstdx = Tok("rstdx")

    st_sem = S.dma_sem("sp")
    id_sem = S.dma_sem("pool")
    t_ident = Tok("ident")
    S.emit("sp", I("dma_start", out=cols[:, :], in_=cols_d[:, :]), dsem=st_sem)
    S.emit("sp", I("dma_start", out=consts[:, :], in_=consts_d[:, 128:128 * 3 + 512]), dsem=st_sem)
    S.emit("pool", I("dma_start", out=ident[:, :], in_=consts_d[:, 0:128]), writes=(t_ident,), dsem=id_sem)
    for i in range(NT):
        S.emit("sp", (lambda i: I("dma_start", out=x_sb[:, i, :], in_=x_d[i * 128:(i + 1) * 128, :]))(i),
               dsem=st_sem)
    st_ev = Ev(st_sem, st_sem.count, {})
    t_const.lw = st_ev
    for t in x_tok:
        t.lw = st_ev

    S.emit("dve", I("memset", ones_bf[:, :], 1.0), writes=(t_const,))
    S.emit("dve", I("memset", ones_f[:, :], 1.0), writes=(t_const,))
    S.emit("act", I("activation", out=c_act[:, :], in_=cols[:, C_C:C_C + 8], func=AF.Silu),
           reads=(t_const,), writes=(t_crep,))
    for k in range(8):
        S.emit("dve", (lambda k: I("tensor_scalar", out=c_rep[:, k, :], in0=ones_f[:, :],
                                                           scalar1=c_act[:, k:k + 1], scalar2=None,
                                                           op0=ALU.mult))(k),
               reads=(t_const,), writes=(t_crep,))
    S.emit("dve", I("memset", c_rep[:, 8, :], 0.0), writes=(t_crep,))
    S.emit("dve", I("memset", c_rep[0:1, 8, :], 1.0), writes=(t_crep,))
    t_lbc = Tok("lbc")
    S.emit("dve", I("memset", lbc[:, 0:4], 1.0), writes=(t_lbc,))
    S.emit("dve", I("memset", lbc[:, 4:8], 0.0), writes=(t_lbc,))
    S.emit("dve", I("memset", lbc[:, 8:12], -1.0), writes=(t_lbc,))
    S.emit("dve", I("tensor_tensor", out=lbc[:, 12:16], in0=cols[:, C_LB + 4:C_LB + 8],
                                            in1=cols[:, C_LB:C_LB + 4], op=ALU.subtract),
           reads=(t_const,), writes=(t_lbc,))
    S.emit("act", I("activation", out=lbc[:, 16:20], in_=lbc[:, 12:16], func=AF.Sigmoid),
           reads=(t_lbc,), writes=(t_lbc,))
    S.emit("dve", I("tensor_scalar", out=lbc[:, 12:16], in0=lbc[:, 16:20], scalar1=-1.0, scalar2=1.0,
                                            op0=ALU.mult, op1=ALU.add),
           reads=(t_lbc,), writes=(t_lbc,))
    S.emit("dve", I("tensor_scalar", out=lbc[:, 20:24], in0=lbc[:, 16:20], scalar1=-1.0, scalar2=None,
                                            op0=ALU.add),
           reads=(t_lbc,), writes=(t_lbc,))

    stage = [0]

    def stage_ok():
        stage[0] += 1
        return stage[0] <= n_stage

    def load_w(dst_ap, src_ap, tok, sem):
        S.emit("pool", I("dma_start", out=dst_ap, in_=src_ap), writes=(tok,), dsem=sem)

    def norm_stats(tiles, junk, t_junk):
        for i in tiles:
            S.emit("act", (lambda i: I("activation", out=junk[:, :], in_=x_sb[:, i, :], func=AF.Square,
                                                            accum_out=ssq[:, i:i + 1]))(i),
                   reads=(x_tok[i],), writes=(t_junk, t_ssq))
        a, b = tiles[0], tiles[-1] + 1
        S.emit("act", I("activation", out=ssq[:, a:b], in_=ssq[:, a:b], func=AF.Ln,
                        scale=1.0 / D, bias=eps_col[:, 0:1]),
               reads=(t_ssq, t_const), writes=(t_ssq,))
        S.emit("act", I("activation", out=rstdx[:, a:b], in_=ssq[:, a:b], func=AF.Exp, scale=-0.5),
               reads=(t_ssq,), writes=(t_rstdx,))

    ACT_CHUNKS = ()

    def norm_group(tiles, hT, t_hT, xn, t_xn, tpn, t_tpn, wcol, shcol):
        def emit_xn(k):
            i = tiles[k]
            S.emit("act", I("activation", out=xn[k % 2][:, :], in_=x_sb[:, i, :], func=AF.Copy,
                            scale=rstdx[:, i:i + 1]),
                   reads=(x_tok[i], t_rstdx), writes=(t_xn[k % 2],))
        emit_xn(0)
        for k in range(len(tiles)):
            col0 = k * 128
            for c in range(KC):
                S.emit("pe", I("transpose", out=tpn[:, c * 128:(c + 1) * 128],
                               in_=xn[k % 2][:, c * 128:(c + 1) * 128], identity=ident[:, :]),
                       reads=(t_xn[k % 2], t_ident), writes=(t_tpn,))
            if k + 1 < len(tiles):
                emit_xn(k + 1)
            for c in range(KC):
                if c not in ACT_CHUNKS:
                    S.emit("dve", I("tensor_scalar", out=hT[:, c, col0:col0 + 128],
                                    in0=tpn[:, c * 128:(c + 1) * 128],
                                    scalar1=wsc[:, wcol + c:wcol + c + 1],
                                    scalar2=modcols[:, shcol + c:shcol + c + 1],
                                    op0=ALU.mult, op1=ALU.add),
                           reads=(t_tpn, t_wsc, t_modcols), writes=(t_hT[k][c],))
                else:
                    S.emit("act", I("activation", out=hT[:, c, col0:col0 + 128],
                                    in_=tpn[:, c * 128:(c + 1) * 128], func=AF.Identity,
                                    scale=wsc[:, wcol + c:wcol + c + 1],
                                    bias=modcols[:, shcol + c:shcol + c + 1]),
                           reads=(t_tpn, t_wsc, t_modcols), writes=(t_hT[k][c],))

    def flat(tt):
        return [t for row in tt for t in row]

    eps_col = sb([128, 1], F32, "eps")
    S.emit("dve", I("memset", eps_col[:, :], EPS), writes=(t_const,))

    def head_norm_gate(o_sb, sq_bf, t_osq, nrm_ps, t_nrm, sd, rstdo, y, t_tmp, gate, gcol0, t_gate, nwcol,
                       og_out, t_og):
        S.emit("pe", I("matmul", nrm_ps[:, :], lhsT=ones_bf[:, :], rhs=sq_bf[:, :], start=True, stop=True),
               reads=(t_osq, t_const), writes=(t_nrm,))
        S.emit("act", I("activation", out=sd[:, :], in_=nrm_ps[:, :], func=AF.Ln, scale=1.0 / 128,
                        bias=eps_col[:, 0:1]),
               reads=(t_nrm, t_const), writes=(t_tmp,))
        S.emit("act", I("activation", out=rstdo[:, :], in_=sd[:, :], func=AF.Exp, scale=-0.5),
               reads=(t_tmp,), writes=(t_tmp,))
        S.emit("dve", I("tensor_tensor", out=y[:, :], in0=o_sb[:, :], in1=rstdo[:, :], op=ALU.mult),
               reads=(t_tmp, t_osq), writes=(t_tmp,))
        S.emit("dve", I("tensor_tensor", out=og_out, in0=y[:, :].rearrange("p (h t) -> p h t", h=4),
                        in1=gate[:, :, gcol0:gcol0 + 128], op=ALU.mult),
               reads=(t_tmp, t_gate), writes=(t_og,))

    for l in range(L):
        if not stage_ok():
            break
        s0 = snap()
        ada = [sb([128, 9, 512], BF16, "ada") for _ in range(2)]
        t_ada = [Tok("ada0"), Tok("ada1")]
        sem_ada = [S.dma_sem(), S.dma_sem()]
        rowbuf = [sb([128, 512], F32, "rowbuf") for _ in range(2)]
        t_row = [Tok("row0"), Tok("row1")]
        mps = [ps(F32, "modps") for _ in range(2)]
        t_mps = [Tok("mps0"), Tok("mps1")]
        colps = ps(F32, "colps")
        t_colps = Tok("colps")
        for n in range(2):
            load_w(ada[n][:, :, :], wada_d[l, n], t_ada[n], sem_ada[n])
        for n in range(12):
            b = n % 2
            for k in range(9):
                S.emit("pe", (lambda k, b: I("matmul", mps[b][:, :], lhsT=c_rep[:, k, :], rhs=ada[b][:, k, :],
                                                              start=(k == 0), stop=(k == 8)))(k, b),
                       reads=(t_crep, t_ada[b]), writes=(t_mps[b],))
            if n + 2 < 12:
                load_w(ada[b][:, :, :], wada_d[l, n + 2], t_ada[b], sem_ada[b])
            vec = n // 2
            if vec in (2, 5):
                gi = 0 if vec == 2 else 1
                S.emit("act", (lambda b, gi, n: I("activation",
                    out=grow[:, gi, (n % 2) * 512:(n % 2) * 512 + 512], in_=mps[b][:, :], func=AF.Copy))(b, gi, n),
                       reads=(t_mps[b],), writes=(t_grow[gi],))
            else:
                vi = {0: 0, 1: 1, 3: 2, 4: 3}[vec]
                S.emit("act", (lambda b: I("activation", out=rowbuf[b][0:1, :], in_=mps[b][0:1, :],
                                                                func=AF.Copy))(b),
                       reads=(t_mps[b],), writes=(t_row[b],))
                for q in range(4):
                    idx = vi * 8 + (n % 2) * 4 + q
                    S.emit("pe", (lambda b, q, idx: I("matmul",
                        colps[:, idx:idx + 1], lhsT=rowbuf[b][0:1, q * 128:(q + 1) * 128], rhs=ones_f[0:1, 0:1],
                        start=True, stop=True))(b, q, idx),
                           reads=(t_row[b], t_const), writes=(t_colps,))
        S.emit("dve", I("tensor_copy", out=modcols[:, :], in_=colps[:, 0:32]),
               reads=(t_colps,), writes=(t_modcols,))
        S.emit("dve", (lambda l: I("scalar_tensor_tensor",
            out=wsc[:, 0:8], in0=modcols[:, 8:16], scalar=1.0, in1=cols[:, l * 24 + C_NW1:l * 24 + C_NW1 + 8],
            op0=ALU.add, op1=ALU.mult))(l), reads=(t_modcols, t_const), writes=(t_wsc,))
        S.emit("dve", (lambda l: I("scalar_tensor_tensor",
            out=wsc[:, 8:16], in0=modcols[:, 24:32], scalar=1.0, in1=cols[:, l * 24 + C_NW2:l * 24 + C_NW2 + 8],
            op0=ALU.add, op1=ALU.mult))(l), reads=(t_modcols, t_const), writes=(t_wsc,))
        S.barrier()
        for s_ in sem_ada:
            S.release_dma_sem(s_)
        restore(s0)

        if not stage_ok():
            break
        sm = snap()
        og_ret = sb([128, 4, T], BF16, "og_ret")
        t_ogret = [Tok("ogret%d" % i) for i in range(NT)]

        s0 = snap()
        wq = sb([128, 8, 1024], BF16, "wq")
        wv = sb([128, 8, 512], BF16, "wv")
        wg = sb([128, 8, 512], BF16, "wg")
        t_wq, t_wv, t_wg = Tok("wq"), Tok("wv"), Tok("wg")
        sems_r = [S.dma_sem() for _ in range(4)]
        load_w(wq[:, :, :], wrqk_d[l], t_wq, sems_r[0])
        load_w(wv[:, :, :], wrv_d[l], t_wv, sems_r[1])
        load_w(wg[:, :, :], wrg_d[l], t_wg, sems_r[2])
        hT = sb([128, 8, 512], BF16, "hT")
        t_hT = [[Tok("hT%d_%d" % (i, c)) for c in range(KC)] for i in range(4)]
        rot = sb([128, 4, 2, 512], F32, "rot")
        t_rot = Tok("rot")
        qk = sb([128, 4, 512], BF16, "qk")
        t_qk = [Tok("qk%d" % i) for i in range(4)]
        t1 = [sb([128, 512], F32, "t1") for _ in range(2)]
        t2 = [sb([128, 512], F32, "t2") for _ in range(2)]
        t_t12 = [Tok("t12a"), Tok("t12b")]
        v_sb = sb([128, 4, 512], BF16, "v_sb")
        t_v = [Tok("v%d" % i) for i in range(4)]
        gate = sb([128, 4, 512], F32, "gate")
        t_gate = Tok("gate")
        kT_sb = [sb([128, 256], BF16, "kT") for _ in range(2)]
        t_kT = [Tok("kT0"), Tok("kT1")]
        msc = [sb([128, 4, 128], BF16, "msc") for _ in range(2)]
        t_msc = [Tok("msc0"), Tok("msc1")]
        R_sb = [sb([128, 2, 128], BF16, "R_sb") for _ in range(3)]
        t_R = [Tok("R0"), Tok("R1"), Tok("R2")]
        sq_bf = [sb([128, 512], BF16, "sq") for _ in range(2)]
        o_sb = [sb([128, 512], F32, "o_sb") for _ in range(2)]
        t_osq = [Tok("osq0"), Tok("osq1")]
        sd = sb([128, 512], F32, "sd")
        rstdo = sb([128, 512], F32, "rstdo")
        yb = sb([128, 512], F32, "y")
        t_tmp = Tok("tmp")
        xn = [sb([128, D], BF16, "xn") for _ in range(2)]
        t_xn = [Tok("xn0"), Tok("xn1")]
        junk = sb([128, D], BF16, "junk")
        t_junk = Tok("junk")
        mmps = [ps(F32, "mm") for _ in range(2)]
        t_mm = [Tok("mm0"), Tok("mm1")]
        tpn = ps(BF16, "tpn")
        t_tpn = Tok("tpn")
        sc_ab = [ps(F32, "sca"), ps(F32, "scb")]
        t_scab = [Tok("sca"), Tok("scb")]
        o_ps = ps(F32, "o")
        t_o = Tok("o")
        R_ps = ps(F32, "Rps")
        t_Rps = Tok("Rps")
        Rst = sb([128, 512], F32, "Rst")
        t_Rst = Tok("Rst")
        S.emit("dve", I("memset", Rst[:, :], 0.0), writes=(t_Rst,))
        nrm_ps = ps(F32, "nrm")
        t_nrm = Tok("nrm")
        tpk = sc_ab[1][:, 256:512].bitcast(BF16)
        t_tpk = t_scab[1]

        load_w(rot[:, :, :, :], rot_d[0], t_rot, sems_r[3])
        for g in range(4):
            norm_stats([g * 4 + i for i in range(4)], junk, t_junk)
            norm_group([g * 4 + i for i in range(4)], hT, t_hT, xn, t_xn, tpn, t_tpn, 0, 0)
            for qi in range(4):
                m = qi % 2
                isk = qi // 2
                ca = isk * 512 + m * 128
                cb_ = isk * 512 + 256 + m * 128
                for pb, c0 in ((0, ca), (1, cb_)):
                    for c in range(KC):
                        S.emit("pe", (lambda pb, c0, c: I("matmul",
                            mmps[pb][:, :], lhsT=wq[:, c, c0:c0 + 128], rhs=hT[:, c, :],
                            start=(c == 0), stop=(c == KC - 1)))(pb, c0, c),
                               reads=(t_wq, *flat(t_hT)), writes=(t_mm[pb],))
                tb = qi % 2
                S.emit("dve", (lambda tb, isk, m: I("tensor_tensor",
                    out=t1[tb][:, :], in0=mmps[0][:, :], in1=rot[:, 2 * isk, m, :], op=ALU.mult))(tb, isk, m),
                       reads=(t_mm[0], t_rot), writes=(t_t12[tb],))
                S.emit("dve", (lambda tb, isk, m: I("tensor_tensor",
                    out=t2[tb][:, :], in0=mmps[1][:, :], in1=rot[:, 2 * isk + 1, m, :], op=ALU.mult))(tb, isk, m),
                       reads=(t_mm[1], t_rot), writes=(t_t12[tb],))
                S.emit("dve", (lambda tb, qi: I("tensor_tensor",
                    out=qk[:, qi, :], in0=t1[tb][:, :], in1=t2[tb][:, :], op=ALU.add))(tb, qi),
                       reads=(t_t12[tb],), writes=(t_qk[qi],))
            if g + 1 < 4:
                load_w(rot[:, :, :, :], rot_d[g + 1], t_rot, sems_r[3])
            for i in range(4):
                pb = i % 2
                for c in range(KC):
                    S.emit("pe", (lambda pb, i, c: I("matmul",
                        mmps[pb][:, :], lhsT=hT[:, c, i * 128:(i + 1) * 128], rhs=wv[:, c, :],
                        start=(c == 0), stop=(c == KC - 1)))(pb, i, c),
                           reads=(t_wv, *t_hT[i]), writes=(t_mm[pb],))
                S.emit("act", (lambda pb, i: I("activation", out=v_sb[:, i, :], in_=mmps[pb][:, :],
                                                                    func=AF.Copy))(pb, i),
                       reads=(t_mm[pb],), writes=(t_v[i],))
            for m in range(4):
                pb = m % 2
                for c in range(KC):
                    S.emit("pe", (lambda pb, m, c: I("matmul",
                        mmps[pb][:, :], lhsT=wg[:, c, m * 128:(m + 1) * 128], rhs=hT[:, c, :],
                        start=(c == 0), stop=(c == KC - 1)))(pb, m, c),
                           reads=(t_wg, *flat(t_hT)), writes=(t_mm[pb],))
                S.emit("act", (lambda pb, m: I("activation", out=gate[:, m, :], in_=mmps[pb][:, :],
                                                                    func=AF.Silu))(pb, m),
                       reads=(t_mm[pb],), writes=(t_gate,))

            def stA(i, g=g):
                b = i % 2
                gi = g * 4 + i
                cs = slice(i * 128, (i + 1) * 128)
                for m in range(2):
                    S.emit("pe", (lambda m: I("transpose", out=tpk[:, m * 128:(m + 1) * 128],
                                                                  in_=qk[:, 2 + m, cs], identity=ident[:, :]))(m),
                           reads=(t_qk[2 + m], t_ident), writes=(t_tpk,))
                S.emit("act", I("activation", out=kT_sb[b][:, :], in_=tpk[:, 0:256], func=AF.Copy),
                       reads=(t_tpk,), writes=(t_kT[b],))
                for h in range(4):
                    p0 = (h % 2) * 64
                    S.emit("pe", (lambda h, p0: I("matmul",
                        sc_ab[h % 2][:, (h // 2) * 128:(h // 2 + 1) * 128], lhsT=qk[p0:p0 + 64, 2 + h // 2, cs],
                        rhs=qk[p0:p0 + 64, h // 2, cs], start=True, stop=True))(h, p0),
                           reads=(t_qk[2 + h // 2], t_qk[h // 2]), writes=(t_scab[h % 2],))
                for par in range(2):
                    S.emit("dve", (lambda par: I("tensor_tensor",
                        out=msc[b][:, 2 * par:2 * par + 2, :],
                        in0=sc_ab[par][:, 0:256].rearrange("p (h t) -> p h t", h=2),
                        in1=mask_ret.unsqueeze(1).broadcast_to([128, 2, 128]), op=ALU.mult))(par),
                           reads=(t_scab[par], t_const), writes=(t_msc[b],))

            def stA2(i, g=g):
                b = i % 2
                gi = g * 4 + i
                if gi < NT - 1:
                    for hp in range(2):
                        S.emit("pe", (lambda hp: I("matmul",
                            R_ps[:, hp * 256:(hp + 1) * 256], lhsT=kT_sb[b][:, hp * 128:(hp + 1) * 128],
                            rhs=v_sb[:, i, hp * 256:(hp + 1) * 256], start=(hp == 0), stop=(hp == 1)))(hp),
                               reads=(t_kT[b], t_v[i]), writes=(t_Rps,))
                    S.emit("dve", I("tensor_tensor", out=Rst[:, :], in0=R_ps[:, :], in1=Rst[:, :], op=ALU.add),
                           reads=(t_Rps, t_Rst), writes=(t_Rst,))
                    for hl in range(2):
                        S.emit("act", I("activation", out=R_sb[(gi + 1) % 3][hl * 64:(hl + 1) * 64, :, :],
                                        in_=Rst[hl * 64:(hl + 1) * 64, :].rearrange(
                                            "p (hp x) -> p hp x", hp=2)[:, :, hl * 128:(hl + 1) * 128],
                                        func=AF.Copy),
                               reads=(t_Rst,), writes=(t_R[(gi + 1) % 3],))

            def stB(i, g=g):
                b = i % 2
                gi = g * 4 + i
                cs = slice(i * 128, (i + 1) * 128)
                for h in range(4):
                    p0 = (h % 2) * 64
                    S.emit("pe", (lambda h: I("matmul",
                        o_ps[:, h * 128:(h + 1) * 128], lhsT=v_sb[:, i, h * 128:(h + 1) * 128],
                        rhs=msc[b][:, (h % 2) * 2 + h // 2, :], start=True, stop=(gi == 0)))(h),
                           reads=(t_v[i], t_msc[b]), writes=(t_o,))
                    if gi > 0:
                        S.emit("pe", (lambda h, p0: I("matmul",
                            o_ps[:, h * 128:(h + 1) * 128], lhsT=R_sb[gi % 3][p0:p0 + 64, h // 2, :],
                            rhs=qk[p0:p0 + 64, h // 2, cs], start=False, stop=True))(h, p0),
                               reads=(t_R[gi % 3], t_qk[h // 2]), writes=(t_o,))
                S.emit("act", I("activation", out=sq_bf[b][:, :], in_=o_ps[:, :], func=AF.Square),
                       reads=(t_o,), writes=(t_osq[b],))
                S.emit("act", I("activation", out=o_sb[b][:, :], in_=o_ps[:, :], func=AF.Copy),
                       reads=(t_o,), writes=(t_osq[b],))

            def stC(i, g=g, l=l):
                b = i % 2
                gi = g * 4 + i
                head_norm_gate(o_sb[b], sq_bf[b], t_osq[b], nrm_ps, t_nrm, sd, rstdo, yb, t_tmp, gate, i * 128,
                               t_gate, l * 24 + C_RNW,
                               og_ret[:, :, gi * 128:(gi + 1) * 128], t_ogret[gi])

            for step in range(4 + 2):
                if step < 4:
                    stA(step)
                if 0 <= step - 2 < 4:
                    stC(step - 2)
                if 0 <= step - 1 < 4:
                    stB(step - 1)
                if step < 4:
                    stA2(step)
        S.barrier()
        for s_ in sems_r:
            S.release_dma_sem(s_)
        restore(s0)

        if not stage_ok():
            restore(sm)
            break
        s0 = snap()
        whq = sb([128, 8, 512], BF16, "whq")
        whf = sb([128, 8, 512], BF16, "whf")
        whi = sb([128, 8, 512], BF16, "whi")
        whg = sb([128, 8, 512], BF16, "whg")
        wo = sb([128, 8, 1024], BF16, "wo")
        t_whq, t_whf, t_whi, t_whg, t_wo = Tok("whq"), Tok("whf"), Tok("whi"), Tok("whg"), Tok("wo")
        sems_h = [S.dma_sem() for _ in range(5)]
        load_w(whi[:, :, :], whi_d[l], t_whi, sems_h[2])
        load_w(whg[:, :, :], whg_d[l], t_whg, sems_h[3])
        load_w(whf[:, :, :], whf_d[l], t_whf, sems_h[0])
        load_w(whq[:, :, :], whq_d[l], t_whq, sems_h[1])
        load_w(wo[:, :, :], wo_d[l], t_wo, sems_h[4])
        hT = sb([128, 8, 512], BF16, "hT")
        t_hT = [[Tok("hT%d_%d" % (i, c)) for c in range(KC)] for i in range(4)]
        B1 = sb([128, 512], F32, "B1")
        B2 = sb([128, 512], F32, "B2")
        B3 = sb([128, 512], F32, "B3")
        B4 = sb([128, 512], F32, "B4")
        t_B1, t_B2, t_B3, t_B4 = Tok("B1"), Tok("B2"), Tok("B3"), Tok("B4")
        Aend = sb([128, 4, 8], F32, "Aend")
        t_A = Tok("Aend")
        qfb = sb([128, 4, 512], BF16, "qfb")
        t_qf = [Tok("qf%d" % i) for i in range(4)]
        qt = sb([128, 4, 512], BF16, "qt")
        t_qt = Tok("qt")
        kt = sb([128, 4, 512], BF16, "kt")
        t_kt = Tok("kt")
        vh = sb([128, 4, 512], BF16, "vh")
        t_vh = [Tok("vh%d" % i) for i in range(4)]
        gate = sb([128, 4, 512], BF16, "gateh")
        t_gate = Tok("gateh")
        kT_sb = [sb([128, 4, 128], BF16, "kTh") for _ in range(2)]
        t_kT = [Tok("kTh0"), Tok("kTh1")]
        msc = [sb([128, 4, 128], BF16, "msch") for _ in range(2)]
        t_msc = [Tok("msch0"), Tok("msch1")]
        Sst = sb([128, 512], F32, "Sst")
        Stmp = sb([128, 512], F32, "Stmp")
        S_ring = [sb([128, 4, 128], BF16, "S_ring") for _ in range(5)]
        t_S = Tok("S")
        t_Sr = [Tok("Sr%d" % i) for i in range(5)]
        sq_bf = [sb([128, 512], BF16, "sqh") for _ in range(2)]
        o_sb = [sb([128, 512], F32, "o_sbh") for _ in range(2)]
        t_osq = [Tok("osqh0"), Tok("osqh1")]
        sd = sb([128, 512], F32, "sdh")
        t_tmp = Tok("tmph")
        og_hg = [sb([128, 4, 128], BF16, "og_hg") for _ in range(2)]
        t_oghg = [Tok("oghg0"), Tok("oghg1")]
        xn = [sb([128, D], BF16, "xnh")] * 2
        t_xn = [Tok("xnh0")] * 2
        mmps = [ps(F32, "mmh") for _ in range(2)]
        t_mm = [Tok("mmh0"), Tok("mmh1")]
        tpn = ps(BF16, "tpnh")
        t_tpn = Tok("tpnh")
        tpk = ps(BF16, "tpkh")
        t_tpk = Tok("tpkh")
        sc_ps = ps(F32, "sch")
        t_sc = Tok("sch")
        o_ps = ps(F32, "oh")
        t_o = Tok("oh")
        P_ps = ps(F32, "Pps")
        t_P = Tok("Pps")
        nrm_ps = ps(F32, "nrmh")
        t_nrm = Tok("nrmh")

        def scale_wo(l=l):
            for kc in range(8):
                nwc = l * 24 + (C_RNW + kc if kc < 4 else C_HNW + kc - 4)
                S.emit("dve", I("scalar_tensor_tensor", out=wo[:, kc, :], in0=wo[:, kc, :],
                                scalar=cols[:, nwc:nwc + 1], in1=grow[:, 0, :], op0=ALU.mult, op1=ALU.mult),
                       reads=(t_wo, t_grow[0], t_const), writes=(t_wo,))
        S.emit("dve", I("memset", Sst[:, :], 0.0), writes=(t_S,))
        S.emit("dve", I("memset", S_ring[0][:, :, :], 0.0), writes=(t_Sr[0],))
        lo = l * 12
        for g in range(4):
            norm_group([g * 4 + i for i in range(4)], hT, t_hT, xn, t_xn, tpn, t_tpn, 0, 0)
            for i in range(4):
                pb = i % 2
                for c in range(KC):
                    S.emit("pe", (lambda pb, i, c: I("matmul",
                        mmps[pb][:, :], lhsT=hT[:, c, i * 128:(i + 1) * 128], rhs=whi[:, c, :],
                        start=(c == 0), stop=(c == KC - 1)))(pb, i, c),
                           reads=(t_whi, *t_hT[i]), writes=(t_mm[pb],))
                S.emit("act", (lambda pb, i: I("activation", out=vh[:, i, :], in_=mmps[pb][:, :],
                                                                    func=AF.Copy))(pb, i),
                       reads=(t_mm[pb],), writes=(t_vh[i],))
            for m in range(4):
                pb = m % 2
                for c in range(KC):
                    S.emit("pe", (lambda pb, m, c: I("matmul",
                        mmps[pb][:, :], lhsT=whg[:, c, m * 128:(m + 1) * 128], rhs=hT[:, c, :],
                        start=(c == 0), stop=(c == KC - 1)))(pb, m, c),
                           reads=(t_whg, *flat(t_hT)), writes=(t_mm[pb],))
                S.emit("act", (lambda pb, m: I("activation", out=gate[:, m, :], in_=mmps[pb][:, :],
                                                                    func=AF.Silu))(pb, m),
                       reads=(t_mm[pb],), writes=(t_gate,))
            for m in range(4):
                pb = m % 2
                for c in range(KC):
                    S.emit("pe", I("matmul", mmps[pb][:, :], lhsT=whq[:, c, m * 128:(m + 1) * 128], rhs=hT[:, c, :],
                                   start=(c == 0), stop=(c == KC - 1)),
                           reads=(t_whq, *flat(t_hT)), writes=(t_mm[pb],))
                S.emit("act", I("activation", out=qfb[:, m, :], in_=mmps[pb][:, :], func=AF.Silu),
                       reads=(t_mm[pb],), writes=(t_qf[m],))
            for m in range(4):
                pb = m % 2
                for c in range(KC):
                    S.emit("pe", I("matmul", mmps[pb][:, :], lhsT=whf[:, c, m * 128:(m + 1) * 128], rhs=hT[:, c, :],
                                   start=(c == 0), stop=(c == KC - 1)),
                           reads=(t_whf, *flat(t_hT)), writes=(t_mm[pb],))
                S.emit("act", I("activation", out=B1[:, :], in_=mmps[pb][:, :], func=AF.Exp, scale=-1.0),
                       reads=(t_mm[pb],), writes=(t_B1,))
                S.emit("act", I("activation", out=B1[:, :], in_=B1[:, :], func=AF.Ln, bias=ones_f[:, 0:1]),
                       reads=(t_B1, t_const), writes=(t_B1,))
                S.emit("act", I("activation", out=B2[:, :], in_=B1[:, :], func=AF.Exp, scale=-1.0),
                       reads=(t_B1,), writes=(t_B2,))
                S.emit("act", I("activation", out=B1[:, :], in_=B2[:, :], func=AF.Ln,
                                scale=lbc[:, lo + m:lo + m + 1], bias=lbc[:, lo + 4 + m:lo + 4 + m + 1]),
                       reads=(t_B2, t_lbc), writes=(t_B1,))
                S.emit("dve", I("tensor_scalar", out=B2[:, :], in0=B2[:, :], scalar1=lbc[:, lo + 8 + m:lo + 8 + m + 1],
                                scalar2=lbc[:, lo + m:lo + m + 1], op0=ALU.mult, op1=ALU.add),
                       reads=(t_B2, t_lbc), writes=(t_B2,))
                S.emit("dve", I("tensor_tensor_scan", out=B3[:, :], data0=resetmask, data1=B1[:, :], initial=0.0,
                                op0=ALU.mult, op1=ALU.add), reads=(t_B1, t_const), writes=(t_B3,))
                S.emit("dve", I("tensor_scalar", out=B3[:, :], in0=B3[:, :], scalar1=-80.0, scalar2=None,
                                op0=ALU.max), reads=(t_B3,), writes=(t_B3,))
                S.emit("act", I("activation", out=B4[:, :], in_=B3[:, :], func=AF.Exp),
                       reads=(t_B3,), writes=(t_B4,))
                S.emit("act", I("activation", out=B1[:, :], in_=B3[:, :], func=AF.Exp, scale=-1.0),
                       reads=(t_B3,), writes=(t_B1,))
                S.emit("dve", I("tensor_tensor", out=kt[:, m, :], in0=B2[:, :], in1=B1[:, :], op=ALU.mult),
                       reads=(t_B2, t_B1), writes=(t_kt,))
                S.emit("dve", I("tensor_copy", out=Aend[:, m, :],
                                in_=B4[:, :].rearrange("p (c t) -> p c t", t=64)[:, :, 63]),
                       reads=(t_B4,), writes=(t_A,))
                S.emit("dve", I("tensor_tensor", out=qt[:, m, :], in0=qfb[:, m, :], in1=B4[:, :], op=ALU.mult),
                       reads=(t_qf[m], t_B4), writes=(t_qt,))

            def stA(i, g=g):
                b = i % 2
                cs = slice(i * 128, (i + 1) * 128)
                for h in range(4):
                    S.emit("pe", (lambda h: I("transpose", out=tpk[:, h * 128:(h + 1) * 128],
                                                                  in_=kt[:, h, cs], identity=ident[:, :]))(h),
                           reads=(t_kt, t_ident), writes=(t_tpk,))
                S.emit("act", I("activation", out=kT_sb[b][:, :, :],
                                                     in_=tpk[:, 0:512].rearrange("p (h t) -> p h t", h=4),
                                                     func=AF.Copy),
                       reads=(t_tpk,), writes=(t_kT[b],))
                for h in range(4):
                    S.emit("pe", (lambda h: I("matmul",
                        sc_ps[:, h * 128:(h + 1) * 128], lhsT=kt[:, h, cs], rhs=qt[:, h, cs],
                        start=True, stop=True))(h),
                           reads=(t_kt, t_qt), writes=(t_sc,))
                S.emit("dve", I("tensor_tensor",
                    out=msc[b][:, :, :], in0=sc_ps[:, :].rearrange("p (h t) -> p h t", h=4),
                    in1=mask_hg.unsqueeze(1).broadcast_to([128, 4, 128]), op=ALU.mult),
                       reads=(t_sc, t_const), writes=(t_msc[b],))

            def stA2(i, g=g):
                b = i % 2
                gi = g * 4 + i
                for j in range(2):
                    r0 = j * 64
                    n = 2 * gi + j
                    for h in range(4):
                        S.emit("pe", (lambda h: I("matmul",
                            P_ps[:, h * 128:(h + 1) * 128], lhsT=kT_sb[b][r0:r0 + 64, h, :],
                            rhs=vh[r0:r0 + 64, i, h * 128:(h + 1) * 128], start=True, stop=True))(h),
                               reads=(t_kT[b], t_vh[i]), writes=(t_P,))
                    S.emit("dve", I("tensor_tensor", out=Stmp[:, :], in0=P_ps[:, :], in1=Sst[:, :], op=ALU.add),
                           reads=(t_P, t_S), writes=(t_S,))
                    S.emit("dve", I("tensor_tensor",
                        out=Sst[:, :].rearrange("p (h v) -> p h v", h=4),
                        in0=Stmp[:, :].rearrange("p (h v) -> p h v", h=4),
                        in1=Aend[:, :, i * 2 + j:i * 2 + j + 1].broadcast_to([128, 4, 128]), op=ALU.mult),
                           reads=(t_S, t_A), writes=(t_S,))
                    S.emit("act", I("activation", out=S_ring[(n + 1) % 5][:, :, :],
                                    in_=Sst[:, :].rearrange("p (h v) -> p h v", h=4), func=AF.Copy),
                           reads=(t_S,), writes=(t_Sr[(n + 1) % 5],))

            def stB(i, g=g):
                b = i % 2
                gi = g * 4 + i
                for h in range(4):
                    S.emit("pe", (lambda h: I("matmul",
                        o_ps[:, h * 128:(h + 1) * 128], lhsT=vh[:, i, h * 128:(h + 1) * 128],
                        rhs=msc[b][:, h, :], start=(h == 0), stop=False))(h),
                           reads=(t_vh[i], t_msc[b]), writes=(t_o,))
                for j in range(2):
                    c0 = i * 128 + j * 64
                    r0 = j * 64
                    n = 2 * gi + j
                    for h in range(4):
                        S.emit("pe", (lambda h: I("matmul",
                            o_ps[:, h * 128 + r0:h * 128 + r0 + 64], lhsT=S_ring[n % 5][:, h, :],
                            rhs=qt[:, h, c0:c0 + 64], start=False, stop=(j == 1 and h == 3)))(h),
                               reads=(t_Sr[n % 5], t_qt), writes=(t_o,))
                S.emit("act", I("activation", out=sq_bf[b][:, :], in_=o_ps[:, :], func=AF.Square),
                       reads=(t_o,), writes=(t_osq[b],))
                S.emit("act", I("activation", out=o_sb[b][:, :], in_=o_ps[:, :], func=AF.Copy),
                       reads=(t_o,), writes=(t_osq[b],))

            def stC(i, g=g, l=l):
                b = i % 2
                gi = g * 4 + i
                head_norm_gate(o_sb[b], sq_bf[b], t_osq[b], nrm_ps, t_nrm, sd, sd, o_sb[b], t_tmp, gate, i * 128,
                               t_gate, l * 24 + C_HNW, og_hg[b][:, :, :], t_oghg[b])

            def stC2(i, g=g):
                b = i % 2
                gi = g * 4 + i
                for nh in range(2):
                    pb = nh
                    for kc in range(8):
                        if kc < 4:
                            lhs = og_ret[:, kc, gi * 128:(gi + 1) * 128]
                            rd = t_ogret[gi]
                        else:
                            lhs = og_hg[b][:, kc - 4, :]
                            rd = t_oghg[b]
                        S.emit("pe", I("matmul", mmps[pb][:, :], lhsT=lhs, rhs=wo[:, kc, nh * 512:(nh + 1) * 512],
                                       start=(kc == 0), stop=(kc == 7)),
                               reads=(rd, t_wo), writes=(t_mm[pb],))
                    S.emit("dve", I("tensor_tensor", out=x_sb[:, gi, nh * 512:(nh + 1) * 512],
                                    in0=mmps[pb][:, :], in1=x_sb[:, gi, nh * 512:(nh + 1) * 512], op=ALU.add),
                           reads=(t_mm[pb], x_tok[gi]), writes=(x_tok[gi],))

            if g == 0:
                scale_wo()
            for step in range(4 + 3):
                if step < 4:
                    stA(step)
                if 0 <= step - 2 < 4:
                    stC(step - 2)
                if 0 <= step - 3 < 4:
                    stC2(step - 3)
                if 0 <= step - 1 < 4:
                    stB(step - 1)
                if step < 4:
                    stA2(step)
        S.barrier()
        for s_ in sems_h:
            S.release_dma_sem(s_)
        restore(s0)
        restore(sm)

        if not stage_ok():
            break
        s0 = snap()
        hT2 = sb([128, 8, T], BF16, "hT2")
        t_hT2 = [[Tok("hT2_%d_%d" % (i, c)) for c in range(KC)] for i in range(NT)]
        a_sb = sb([128, 11, 1024], BF16, "a")
        t_a = [Tok("a%d" % j) for j in range(11)]
        NWS = 3
        wgu = [sb([128, 2, 8, 128], BF16, "wgu") for _ in range(NWS)]
        t_wgu = [Tok("wgu%d" % i) for i in range(NWS)]
        sem_wgu = [S.dma_sem() for _ in range(NWS)]
        NWD = 3
        wdb = [sb([128, 11, 512], BF16, "wd") for _ in range(NWD)]
        t_wd = [Tok("wd%d" % i) for i in range(NWD)]
        sem_wd = [S.dma_sem() for _ in range(NWD)]
        sg = [sb([128, 512], F32, "sg") for _ in range(2)]
        t_sg = [Tok("sg0"), Tok("sg1")]
        rtmp = [sb([128, 512], F32, "rtmpf") for _ in range(2)]
        t_rtmp = [Tok("rtmpf0"), Tok("rtmpf1")]
        xn = [sb([128, D], BF16, "xnf") for _ in range(2)]
        t_xn = [Tok("xnf0"), Tok("xnf1")]
        junk = sb([128, D], BF16, "junkf")
        t_junk = Tok("junkf")
        gps = [ps(F32, "gps") for _ in range(2)]
        ups = [ps(F32, "ups") for _ in range(2)]
        t_gps = [Tok("gps0"), Tok("gps1")]
        t_ups = [Tok("ups0"), Tok("ups1")]
        tpn = ps(BF16, "tpnf")
        t_tpn = Tok("tpnf")
        dps = [ps(F32, "dps") for _ in range(2)]
        t_dps = [Tok("dps0"), Tok("dps1")]

        slab_seq = [j for _th in range(2) for j in range(NJ)]
        wd_seq = [(ffh, nh) for _th in range(2) for ffh in range(2) for nh in range(2)]

        def load_slab(q, l=l):
            j = slab_seq[q]
            slot = q % NWS
            S.emit("pool", I("dma_start", out=wgu[slot][:, 0, :, :], in_=wgs_d[l, j]),
                   writes=(t_wgu[slot],), dsem=sem_wgu[slot])
            S.emit("pool", I("dma_start", out=wgu[slot][:, 1, :, :], in_=wus_d[l, j]),
                   writes=(t_wgu[slot],), dsem=sem_wgu[slot])

        def load_wd(q, l=l):
            ffh, nh = wd_seq[q]
            slot = q % NWD
            S.emit("pool", I("dma_start", out=wdb[slot][:, :, :],
                             in_=wd_d[l, :, ffh * 11:(ffh + 1) * 11, nh * 512:(nh + 1) * 512]),
                   writes=(t_wd[slot],), dsem=sem_wd[slot])

        for q in range(NWS):
            load_slab(q)
        for q in range(NWD):
            load_wd(q)

        def ffn_norm(th):
            tiles = list(range(th * 8, th * 8 + 8))
            norm_stats(tiles, junk, t_junk)
            norm_group(tiles, hT2[:, :, th * 1024:(th + 1) * 1024], t_hT2[th * 8:(th + 1) * 8],
                       xn, t_xn, tpn, t_tpn, 8, 16)

        ffn_norm(0)
        ev_ctr = 0
        sq = 0
        wq_i = 0
        for th in range(2):
            tiles = list(range(th * 8, th * 8 + 8))
            for ffh in range(2):
                for jj in range(11):
                    slot = sq % NWS
                    for g2 in range(2):
                        pb = (sq * 2 + g2) % 2
                        c0 = th * 1024 + g2 * 512
                        rd_h = flat(t_hT2[th * 8 + g2 * 4:th * 8 + g2 * 4 + 4])
                        for c in range(KC):
                            S.emit("pe", I("matmul", gps[pb][:, :], lhsT=wgu[slot][:, 0, c, :],
                                           rhs=hT2[:, c, c0:c0 + 512], start=(c == 0), stop=(c == KC - 1)),
                                   reads=(t_wgu[slot], *rd_h), writes=(t_gps[pb],))
                        for c in range(KC):
                            S.emit("pe", I("matmul", ups[pb][:, :], lhsT=wgu[slot][:, 1, c, :],
                                           rhs=hT2[:, c, c0:c0 + 512], start=(c == 0), stop=(c == KC - 1)),
                                   reads=(t_wgu[slot], *rd_h), writes=(t_ups[pb],))
                        S.emit("act", I("activation", out=sg[pb][:, :], in_=gps[pb][:, :], func=AF.Silu),
                               reads=(t_gps[pb],), writes=(t_sg[pb],))
                        S.emit("dve", I("tensor_tensor", out=a_sb[:, jj, g2 * 512:(g2 + 1) * 512],
                                        in0=sg[pb][:, :], in1=ups[pb][:, :], op=ALU.mult),
                               reads=(t_sg[pb], t_ups[pb]), writes=(t_a[jj],))
                    if sq + NWS < len(slab_seq):
                        load_slab(sq + NWS)
                    sq += 1
                if th == 0 and ffh == 0:
                    ffn_norm(1)
                for nh in range(2):
                    slot = wq_i % NWD
                    for ii, gi in enumerate(tiles):
                        pb = ev_ctr % 2
                        ev_ctr += 1
                        for jj in range(11):
                            S.emit("pe", I("matmul", dps[pb][:, :], lhsT=a_sb[:, jj, ii * 128:(ii + 1) * 128],
                                           rhs=wdb[slot][:, jj, :], start=(jj == 0), stop=(jj == 10)),
                                   reads=(t_a[jj], t_wd[slot]), writes=(t_dps[pb],))
                        S.emit("dve", I("tensor_tensor", out=rtmp[pb][:, :], in0=dps[pb][:, :],
                                        in1=grow[:, 1, nh * 512:(nh + 1) * 512], op=ALU.mult),
                               reads=(t_dps[pb], t_grow[1]), writes=(t_rtmp[pb],))
                        S.emit("dve", I("tensor_tensor", out=x_sb[:, gi, nh * 512:(nh + 1) * 512],
                                        in0=x_sb[:, gi, nh * 512:(nh + 1) * 512], in1=rtmp[pb][:, :], op=ALU.add),
                               reads=(t_rtmp[pb], x_tok[gi]), writes=(x_tok[gi],))
                    if wq_i + NWD < len(wd_seq):
                        load_wd(wq_i + NWD)
                    wq_i += 1
        S.barrier()
        for s_ in sem_wgu + sem_wd:
            S.release_dma_sem(s_)
        restore(s0)

    final = stage[0] <= n_stage and n_stage >= 99
    s0 = snap()
    fw = sb([128, D], F32, "fw")
    t_fw = Tok("fw")
    sem_fw = S.dma_sem("sp")
    S.emit("sp", I("dma_start", out=fw[:, :], in_=fw_d[:, :]), writes=(t_fw,), dsem=sem_fw)
    obuf = [sb([128, D], F32, "obuf") for _ in range(2)]
    t_ob = [Tok("ob0"), Tok("ob1")]
    sem_out = [S.dma_sem("sp"), S.dma_sem("sp")]
    junk = sb([128, D], BF16, "junko")
    t_junk = Tok("junko")
    if final:
        norm_stats(list(range(NT)), junk, t_junk)
    for i in range(NT):
        b = i % 2
        if final:
            S.emit("dve", (lambda i, b: I("scalar_tensor_tensor",
                out=obuf[b][:, :], in0=x_sb[:, i, :], scalar=rstdx[:, i:i + 1], in1=fw[:, :],
                op0=ALU.mult, op1=ALU.mult))(i, b), reads=(x_tok[i], t_rstdx, t_fw), writes=(t_ob[b],))
            S.emit("sp", (lambda i, b: I("dma_start", out=out_d[i * 128:(i + 1) * 128, :],
                                                             in_=obuf[b][:, :]))(i, b),
                   reads=(t_ob[b],), dsem=sem_out[b])
        else:
            S.emit("sp", (lambda i: I("dma_start", out=out_d[i * 128:(i + 1) * 128, :],
                                                          in_=x_sb[:, i, :]))(i),
                   reads=(x_tok[i],), dsem=sem_out[b])
    for b in range(2):
        t = Tok()
        t.lw = Ev(sem_out[b], sem_out[b].count, {})
        S.emit("sp", None, reads=(t,))
    restore(s0)

    with nc.Block() as block:
        @block.tensor
        def _(e):
            S.replay("pe", e)

        @block.scalar
        def _(e):
            S.replay("act", e)

        @block.vector
        def _(e):
            S.replay("dve", e)

        @block.gpsimd
        def _(e):
            S.replay("pool", e)

        @block.sync
        def _(e):
            S.replay("sp", e)
    return nc


def _rot_tables():
    H, DK = 4, 64
    inv = (10000.0 ** (-np.linspace(0.0, 1.0, DK // 2, dtype=np.float32))).astype(np.float32)
    pos = np.arange(T, dtype=np.float32)
    theta = (pos[:, None] * inv[None, :]).astype(np.float32).astype(np.float64)
    cos = np.cos(theta)
    sin = np.sin(theta)
    lg = np.log(1.0 - np.power(2.0, -5.0 - np.arange(H, dtype=np.float64)))
    tt = np.arange(T, dtype=np.float64)
    tabs = np.zeros((4, 128, 2, T), dtype=np.float64)
    for m in range(2):
        for p in range(128):
            h = 2 * m + p // 64
            dd = p % 64
            i = dd // 2
            sgn = -1.0 if dd % 2 == 0 else 1.0
            dq = np.exp(lg[h] * tt)
            dk = np.exp(-lg[h] * tt) * (DK ** -0.5)
            tabs[0, p, m] = cos[:, i] * dq
            tabs[1, p, m] = sgn * sin[:, i] * dq
            tabs[2, p, m] = cos[:, i] * dk
            tabs[3, p, m] = sgn * sin[:, i] * dk
    tabs = tabs.reshape(4, 128, 2, 4, 512).transpose(3, 1, 0, 2, 4)
    return np.ascontiguousarray(tabs.astype(np.float32))


def _consts():
    c = np.zeros((128, 128 * 3 + 512), dtype=np.float32)
    c[:, 0:128] = np.eye(128, dtype=np.float32)
    s = np.arange(128)[:, None]
    t = np.arange(128)[None, :]
    c[:, 128:256] = (s <= t).astype(np.float32)
    c[:, 256:384] = ((s <= t) & (s // 64 == t // 64)).astype(np.float32)
    rm = np.ones(512, dtype=np.float32)
    rm[0::64] = 0.0
    c[:, 384:896] = rm[None, :]
    return c


def _pcn(w):
    K, N = w.shape
    return np.ascontiguousarray(w.reshape(K // 128, 128, N).transpose(1, 0, 2))


_NC_CACHE = {}


def _prep_shared(w_ada, b_ada, w_in, w_out, w_ffn_gate, w_ffn_up, w_ffn_down, final_norm_w):
    f = np.float32
    sh = {}
    ada = np.zeros((L, 1152, 6 * D), dtype=f)
    ada[:, :D] = w_ada
    ada[:, D] = b_ada
    sh["w_ada"] = np.ascontiguousarray(ada.reshape(L, 9, 128, 12, 512).transpose(0, 3, 2, 1, 4))
    swap = np.arange(256) ^ 1
    rq = w_in[:, :, 0:256]
    rk = w_in[:, :, 256:512]
    rqk = np.concatenate([rq, rq[:, :, swap], rk, rk[:, :, swap]], axis=2)
    sh["w_rqk"] = np.stack([_pcn(rqk[l]) for l in range(L)])
    names = [("w_rv", 512), ("w_rg", 1024), ("w_hq", 1536), ("w_hf", 2048), ("w_hi", 2560), ("w_hg", 3072)]
    for nm, o in names:
        sh[nm] = np.stack([_pcn(w_in[l][:, o:o + 512]) for l in range(L)])
    sh["w_o"] = np.stack([_pcn(w_out[l]) for l in range(L)])
    sh["w_gs"] = np.ascontiguousarray(w_ffn_gate.reshape(L, 8, 128, NJ, 128).transpose(0, 3, 2, 1, 4))
    sh["w_us"] = np.ascontiguousarray(w_ffn_up.reshape(L, 8, 128, NJ, 128).transpose(0, 3, 2, 1, 4))
    sh["w_d"] = np.ascontiguousarray(w_ffn_down.reshape(L, NJ, 128, D).transpose(0, 2, 1, 3))
    sh["fwrow"] = np.ascontiguousarray(np.broadcast_to(final_norm_w[None, :], (128, D))).astype(f)
    sh["rot"] = _rot_tables()
    sh["consts"] = _consts()
    return sh


def _cols_for(b, c, norm_mix_w, norm_ffn_w, ret_norm_w, hg_norm_w, hg_lower_bounds):
    cols = np.zeros((128, NCOLS), dtype=np.float32)
    for l in range(L):
        cols[:, l * 24 + C_NW1:l * 24 + C_NW1 + 8] = norm_mix_w[l].reshape(8, 128).T
        cols[:, l * 24 + C_NW2:l * 24 + C_NW2 + 8] = norm_ffn_w[l].reshape(8, 128).T
        cols[:, l * 24 + C_RNW:l * 24 + C_RNW + 4] = ret_norm_w[l].reshape(4, 128).T
        cols[:, l * 24 + C_HNW:l * 24 + C_HNW + 4] = hg_norm_w[l].reshape(4, 128).T
    cols[:, C_LB:C_LB + 4] = hg_lower_bounds[0].reshape(4, 128).T
    cols[:, C_LB + 4:C_LB + 8] = hg_lower_bounds[1].reshape(4, 128).T
    cols[:, C_C:C_C + 8] = c[b].reshape(8, 128).T
    return cols


def kernel(x, c, w_ada, b_ada, norm_mix_w, w_in, ret_norm_w, hg_lower_bounds, hg_norm_w, w_out,
           norm_ffn_w, w_ffn_gate, w_ffn_up, w_ffn_down, final_norm_w, _n_stage=99):
    arrs = [np.asarray(a, dtype=np.float32) for a in
            (x, c, w_ada, b_ada, norm_mix_w, w_in, ret_norm_w, hg_lower_bounds, hg_norm_w, w_out,
             norm_ffn_w, w_ffn_gate, w_ffn_up, w_ffn_down, final_norm_w)]
    (x, c, w_ada, b_ada, norm_mix_w, w_in, ret_norm_w, hg_lower_bounds, hg_norm_w, w_out,
     norm_ffn_w, w_ffn_gate, w_ffn_up, w_ffn_down, final_norm_w) = arrs
    shared = _prep_shared(w_ada, b_ada, w_in, w_out, w_ffn_gate, w_ffn_up, w_ffn_down, final_norm_w)
    if _n_stage not in _NC_CACHE:
        _NC_CACHE[_n_stage] = build_nc(_n_stage)
    nc = _NC_CACHE[_n_stage]
    in_maps = []
    for b in range(8):
        m = dict(shared)
        m["x"] = np.ascontiguousarray(x[b])
        m["cols"] = _cols_for(b, c, norm_mix_w, norm_ffn_w, ret_norm_w, hg_norm_w, hg_lower_bounds)
        in_maps.append(m)
    res = run_bass_kernel_spmd(nc, in_maps, core_ids=list(range(8)))
    return np.stack([np.asarray(r["out"], dtype=np.float32).reshape(T, D) for r in res.results], axis=0)
```

```python
import numpy as np
import concourse.bass as bass
import concourse.mybir as mybir
from concourse.bass_utils import run_bass_kernel_spmd

F32 = mybir.dt.float32
BF16 = mybir.dt.bfloat16
AF = mybir.ActivationFunctionType
ALU = mybir.AluOpType

D = 1024
T = 2048
NT = 16
KC = 8
DFF = 2816
NJ = 22
L = 2
EPS = 1e-6
DEBUG_MEM = False
SAME_ENGINE_SYNC = True

C_NW1 = 0
C_NW2 = 8
C_RNW = 16
C_HNW = 20
C_LB = 48
C_C = 56
NCOLS = 64


def I(name, *args, **kwargs):
    return (name, args, kwargs)


class Sem:
    def __init__(self, handle, name):
        self.h = handle
        self.name = name
        self.count = 0


class Ev:
    __slots__ = ("sem", "val", "clock")

    def __init__(self, sem, val, clock):
        self.sem = sem
        self.val = val
        self.clock = clock


class Tok:
    __slots__ = ("name", "lw", "rd")

    def __init__(self, name=""):
        self.name = name
        self.lw = None
        self.rd = []


class Sched:
    ENGS = ("pe", "act", "dve", "pool", "sp")

    def __init__(self, nc):
        self.nc = nc
        self.sem = {e: Sem(nc.alloc_semaphore("cnt_" + e), "cnt_" + e) for e in self.ENGS}
        self.known = {e: {} for e in self.ENGS}
        self.prog = {e: [] for e in self.ENGS}
        self.free_dma_sems = {}
        self.n_dma_sems = 0

    def dma_sem(self, q="pool"):
        fl = self.free_dma_sems.setdefault(q, [])
        if fl:
            return fl.pop()
        self.n_dma_sems += 1
        name = "dma%s%d" % (q, self.n_dma_sems)
        s = Sem(self.nc.alloc_semaphore(name), name)
        s.q = q
        return s

    def release_dma_sem(self, s):
        self.free_dma_sems[s.q].append(s)

    def emit(self, eng, fn, reads=(), writes=(), dsem=None):
        evs = []
        for t in reads:
            if t.lw is not None:
                evs.append(t.lw)
        for t in writes:
            if t.lw is not None:
                evs.append(t.lw)
            evs.extend(t.rd)
        known = self.known[eng]
        own = self.sem[eng]
        need = {}
        for ev in evs:
            if ev.sem is own and (eng == "pe" or not SAME_ENGINE_SYNC):
                continue
            if known.get(ev.sem.name, 0) >= ev.val:
                continue
            cur = need.get(ev.sem.name)
            if cur is None or cur.val < ev.val:
                need[ev.sem.name] = ev
        waits = []
        for name, ev in need.items():
            waits.append((ev.sem.h, ev.val))
            for k, v in ev.clock.items():
                if known.get(k, 0) < v:
                    known[k] = v
            if known.get(name, 0) < ev.val:
                known[name] = ev.val
        if fn is None:
            self.prog[eng].append((waits, None, None))
            return None
        if dsem is not None:
            dsem.count += 16
            clock = dict(known)
            clock[own.name] = own.count
            ev = Ev(dsem, dsem.count, clock)
            inc = (dsem.h, 16)
        else:
            own.count += 1
            clock = dict(known)
            clock[own.name] = own.count
            ev = Ev(own, own.count, clock)
            inc = (own.h, 1)
        for t in writes:
            t.lw = ev
            t.rd = []
        for t in reads:
            t.rd.append(ev)
        self.prog[eng].append((waits, fn, inc))
        return ev

    def barrier(self):
        evs = {}
        for e in self.ENGS:
            s = self.sem[e]
            if s.count > 0:
                clock = dict(self.known[e])
                clock[s.name] = s.count
                evs[e] = Ev(s, s.count, clock)
        for e in self.ENGS:
            t = Tok()
            for e2, ev in evs.items():
                if e2 == e:
                    continue
                t.lw = ev
                self.emit(e, None, reads=(t,))

    def replay(self, eng, engine_obj):
        for waits, fn, inc in self.prog[eng]:
            for (h, v) in waits:
                engine_obj.wait_ge(h, v)
            if fn is not None:
                ins = getattr(engine_obj, fn[0])(*fn[1], **fn[2])
                ins.then_inc(inc[0], inc[1])


def build_nc(n_stage=99):
    nc = bass.Bass("TRN2", target_bir_lowering=False)
    S = Sched(nc)

    def dram(name, shape, kind="ExternalInput"):
        return nc.dram_tensor(name, list(shape), F32, kind=kind).ap()

    x_d = dram("x", [T, D])
    cols_d = dram("cols", [128, NCOLS])
    consts_d = dram("consts", [128, 128 * 3 + 512])
    fw_d = dram("fwrow", [128, D])
    rot_d = dram("rot", [4, 128, 4, 2, 512])
    wada_d = dram("w_ada", [L, 12, 128, 9, 512])
    wrqk_d = dram("w_rqk", [L, 128, 8, 1024])
    wrv_d = dram("w_rv", [L, 128, 8, 512])
    wrg_d = dram("w_rg", [L, 128, 8, 512])
    whq_d = dram("w_hq", [L, 128, 8, 512])
    whf_d = dram("w_hf", [L, 128, 8, 512])
    whi_d = dram("w_hi", [L, 128, 8, 512])
    whg_d = dram("w_hg", [L, 128, 8, 512])
    wo_d = dram("w_o", [L, 128, 8, 1024])
    wgs_d = dram("w_gs", [L, NJ, 128, 8, 128])
    wus_d = dram("w_us", [L, NJ, 128, 8, 128])
    wd_d = dram("w_d", [L, 128, NJ, 1024])
    out_d = dram("out", [T, D], kind="ExternalOutput")

    cnt = [0]

    def sb(shape, dt=F32, name=None):
        cnt[0] += 1
        return nc.alloc_sbuf_tensor("%s_%d" % (name or "sb", cnt[0]), list(shape), dt)

    def ps(dt=F32, name=None):
        cnt[0] += 1
        shape = [128, 512] if dt == F32 else [128, 1024]
        return nc.alloc_psum_tensor("%s_%d" % (name or "ps", cnt[0]), shape, dt)

    def snap():
        return (nc.sbuf_base, nc.sbuf_top, nc.psum_base, nc.psum_top)

    def restore(s):
        if DEBUG_MEM:
            print("sbuf bytes remaining at scope end:", nc.sbuf_bytes_remaining)
        nc.sbuf_base, nc.sbuf_top, nc.psum_base, nc.psum_top = s

    x_sb = sb([128, NT, D], F32, "x")
    x_tok = [Tok("x%d" % i) for i in range(NT)]
    cols = sb([128, NCOLS], F32, "cols")
    consts = sb([128, 128 * 2 + 512], F32, "consts")
    ident = sb([128, 128], BF16, "ident")
    ones_bf = sb([128, 128], BF16, "ones_bf")
    ones_f = sb([128, 128], F32, "ones_f")
    grow = sb([128, 2, D], F32, "grow")
    modcols = sb([128, 32], F32, "modcols")
    wsc = sb([128, 16], F32, "wsc")
    lbc = sb([128, 24], F32, "lbc")
    c_act = sb([128, 8], F32, "c_act")
    c_rep = sb([128, 9, 128], BF16, "c_rep")
    ssq = sb([128, NT], F32, "ssq")
    rstdx = sb([128, NT], F32, "rstdx")
    mask_ret = consts[:, 0:128]
    mask_hg = consts[:, 128:256]
    resetmask = consts[:, 256:768]

    t_const = Tok("const")
    t_grow = [Tok("g1"), Tok("g2")]
    t_modcols = Tok("modcols")
    t_wsc = Tok("wsc")
    t_crep = Tok("crep")
    t_ssq = Tok("ssq")
    t_rstdx = Tok("rstdx")

    st_sem = S.dma_sem("sp")
    id_sem = S.dma_sem("pool")
    t_ident = Tok("ident")
    S.emit("sp", I("dma_start", out=cols[:, :], in_=cols_d[:, :]), dsem=st_sem)
    S.emit("sp", I("dma_start", out=consts[:, :], in_=consts_d[:, 128:128 * 3 + 512]), dsem=st_sem)
    S.emit("pool", I("dma_start", out=ident[:, :], in_=consts_d[:, 0:128]), writes=(t_ident,), dsem=id_sem)
    for i in range(NT):
        S.emit("sp", (lambda i: I("dma_start", out=x_sb[:, i, :], in_=x_d[i * 128:(i + 1) * 128, :]))(i),
               dsem=st_sem)
    st_ev = Ev(st_sem, st_sem.count, {})
    t_const.lw = st_ev
    for t in x_tok:
        t.lw = st_ev

    S.emit("dve", I("memset", ones_bf[:, :], 1.0), writes=(t_const,))
    S.emit("dve", I("memset", ones_f[:, :], 1.0), writes=(t_const,))
    S.emit("act", I("activation", out=c_act[:, :], in_=cols[:, C_C:C_C + 8], func=AF.Silu),
           reads=(t_const,), writes=(t_crep,))
    for k in range(8):
        S.emit("dve", (lambda k: I("tensor_scalar", out=c_rep[:, k, :], in0=ones_f[:, :],
                                                           scalar1=c_act[:, k:k + 1], scalar2=None,
                                                           op0=ALU.mult))(k),
               reads=(t_const,), writes=(t_crep,))
    S.emit("dve", I("memset", c_rep[:, 8, :], 0.0), writes=(t_crep,))
    S.emit("dve", I("memset", c_rep[0:1, 8, :], 1.0), writes=(t_crep,))
    t_lbc = Tok("lbc")
    S.emit("dve", I("memset", lbc[:, 0:4], 1.0), writes=(t_lbc,))
    S.emit("dve", I("memset", lbc[:, 4:8], 0.0), writes=(t_lbc,))
    S.emit("dve", I("memset", lbc[:, 8:12], -1.0), writes=(t_lbc,))
    S.emit("dve", I("tensor_tensor", out=lbc[:, 12:16], in0=cols[:, C_LB + 4:C_LB + 8],
                                            in1=cols[:, C_LB:C_LB + 4], op=ALU.subtract),
           reads=(t_const,), writes=(t_lbc,))
    S.emit("act", I("activation", out=lbc[:, 16:20], in_=lbc[:, 12:16], func=AF.Sigmoid),
           reads=(t_lbc,), writes=(t_lbc,))
    S.emit("dve", I("tensor_scalar", out=lbc[:, 12:16], in0=lbc[:, 16:20], scalar1=-1.0, scalar2=1.0,
                                            op0=ALU.mult, op1=ALU.add),
           reads=(t_lbc,), writes=(t_lbc,))
    S.emit("dve", I("tensor_scalar", out=lbc[:, 20:24], in0=lbc[:, 16:20], scalar1=-1.0, scalar2=None,
                                            op0=ALU.add),
           reads=(t_lbc,), writes=(t_lbc,))

    stage = [0]

    def stage_ok():
        stage[0] += 1
        return stage[0] <= n_stage

    def load_w(dst_ap, src_ap, tok, sem):
        S.emit("pool", I("dma_start", out=dst_ap, in_=src_ap), writes=(tok,), dsem=sem)

    def norm_stats(tiles, junk, t_junk):
        for i in tiles:
            S.emit("act", (lambda i: I("activation", out=junk[:, :], in_=x_sb[:, i, :], func=AF.Square,
                                                            accum_out=ssq[:, i:i + 1]))(i),
                   reads=(x_tok[i],), writes=(t_junk, t_ssq))
        a, b = tiles[0], tiles[-1] + 1
        S.emit("act", I("activation", out=ssq[:, a:b], in_=ssq[:, a:b], func=AF.Ln,
                        scale=1.0 / D, bias=eps_col[:, 0:1]),
               reads=(t_ssq, t_const), writes=(t_ssq,))
        S.emit("act", I("activation", out=rstdx[:, a:b], in_=ssq[:, a:b], func=AF.Exp, scale=-0.5),
               reads=(t_ssq,), writes=(t_rstdx,))

    ACT_CHUNKS = ()

    def norm_group(tiles, hT, t_hT, xn, t_xn, tpns, t_tpns, wcol, shcol):
        def emit_xn(k):
            i = tiles[k]
            S.emit("act", I("activation", out=xn[k % 2][:, :], in_=x_sb[:, i, :], func=AF.Copy,
                            scale=rstdx[:, i:i + 1]),
                   reads=(x_tok[i], t_rstdx), writes=(t_xn[k % 2],))
        emit_xn(0)
        for k in range(len(tiles)):
            col0 = k * 128
            tpn, t_tpn = tpns[k % len(tpns)], t_tpns[k % len(tpns)]
            for c in range(KC):
                S.emit("pe", I("transpose", out=tpn[:, c * 128:(c + 1) * 128],
                               in_=xn[k % 2][:, c * 128:(c + 1) * 128], identity=ident[:, :]),
                       reads=(t_xn[k % 2], t_ident), writes=(t_tpn,))
            if k + 1 < len(tiles):
                emit_xn(k + 1)
            for c in range(KC):
                if c not in ACT_CHUNKS:
                    S.emit("dve", I("tensor_scalar", out=hT[:, c, col0:col0 + 128],
                                    in0=tpn[:, c * 128:(c + 1) * 128],
                                    scalar1=wsc[:, wcol + c:wcol + c + 1],
                                    scalar2=modcols[:, shcol + c:shcol + c + 1],
                                    op0=ALU.mult, op1=ALU.add),
                           reads=(t_tpn, t_wsc, t_modcols), writes=(t_hT[k][c],))
                else:
                    S.emit("act", I("activation", out=hT[:, c, col0:col0 + 128],
                                    in_=tpn[:, c * 128:(c + 1) * 128], func=AF.Identity,
                                    scale=wsc[:, wcol + c:wcol + c + 1],
                                    bias=modcols[:, shcol + c:shcol + c + 1]),
                           reads=(t_tpn, t_wsc, t_modcols), writes=(t_hT[k][c],))

    def flat(tt):
        return [t for row in tt for t in row]

    eps_col = sb([128, 1], F32, "eps")
    S.emit("dve", I("memset", eps_col[:, :], EPS), writes=(t_const,))

    def head_norm_gate(o_sb, sq_bf, t_osq, nrm_ps, t_nrm, sd, rstdo, y, t_tmp, gate, gcol0, t_gate, nwcol,
                       og_out, t_og):
        S.emit("pe", I("matmul", nrm_ps[:, :], lhsT=ones_bf[:, :], rhs=sq_bf[:, :], start=True, stop=True),
               reads=(t_osq, t_const), writes=(t_nrm,))
        S.emit("act", I("activation", out=sd[:, :], in_=nrm_ps[:, :], func=AF.Ln, scale=1.0 / 128,
                        bias=eps_col[:, 0:1]),
               reads=(t_nrm, t_const), writes=(t_tmp,))
        S.emit("act", I("activation", out=rstdo[:, :], in_=sd[:, :], func=AF.Exp, scale=-0.5),
               reads=(t_tmp,), writes=(t_tmp,))
        S.emit("dve", I("tensor_tensor", out=y[:, :], in0=o_sb[:, :], in1=rstdo[:, :], op=ALU.mult),
               reads=(t_tmp, t_osq), writes=(t_tmp,))
        S.emit("dve", I("tensor_tensor", out=og_out, in0=y[:, :].rearrange("p (h t) -> p h t", h=4),
                        in1=gate[:, :, gcol0:gcol0 + 128], op=ALU.mult),
               reads=(t_tmp, t_gate), writes=(t_og,))

    for l in range(L):
        if not stage_ok():
            break
        s0 = snap()
        ada = [sb([128, 9, 512], BF16, "ada") for _ in range(2)]
        t_ada = [Tok("ada0"), Tok("ada1")]
        sem_ada = [S.dma_sem(), S.dma_sem()]
        rowbuf = [sb([128, 512], F32, "rowbuf") for _ in range(2)]
        t_row = [Tok("row0"), Tok("row1")]
        mps = [ps(F32, "modps") for _ in range(2)]
        t_mps = [Tok("mps0"), Tok("mps1")]
        colps = ps(F32, "colps")
        t_colps = Tok("colps")
        for n in range(2):
            load_w(ada[n][:, :, :], wada_d[l, n], t_ada[n], sem_ada[n])
        for n in range(12):
            b = n % 2
            for k in range(9):
                S.emit("pe", (lambda k, b: I("matmul", mps[b][:, :], lhsT=c_rep[:, k, :], rhs=ada[b][:, k, :],
                                                              start=(k == 0), stop=(k == 8)))(k, b),
                       reads=(t_crep, t_ada[b]), writes=(t_mps[b],))
            if n + 2 < 12:
                load_w(ada[b][:, :, :], wada_d[l, n + 2], t_ada[b], sem_ada[b])
            vec = n // 2
            if vec in (2, 5):
                gi = 0 if vec == 2 else 1
                S.emit("act", (lambda b, gi, n: I("activation",
                    out=grow[:, gi, (n % 2) * 512:(n % 2) * 512 + 512], in_=mps[b][:, :], func=AF.Copy))(b, gi, n),
                       reads=(t_mps[b],), writes=(t_grow[gi],))
            else:
                vi = {0: 0, 1: 1, 3: 2, 4: 3}[vec]
                S.emit("act", (lambda b: I("activation", out=rowbuf[b][0:1, :], in_=mps[b][0:1, :],
                                                                func=AF.Copy))(b),
                       reads=(t_mps[b],), writes=(t_row[b],))
                for q in range(4):
                    idx = vi * 8 + (n % 2) * 4 + q
                    S.emit("pe", (lambda b, q, idx: I("matmul",
                        colps[:, idx:idx + 1], lhsT=rowbuf[b][0:1, q * 128:(q + 1) * 128], rhs=ones_f[0:1, 0:1],
                        start=True, stop=True))(b, q, idx),
                           reads=(t_row[b], t_const), writes=(t_colps,))
        S.emit("dve", I("tensor_copy", out=modcols[:, :], in_=colps[:, 0:32]),
               reads=(t_colps,), writes=(t_modcols,))
        S.emit("dve", (lambda l: I("scalar_tensor_tensor",
            out=wsc[:, 0:8], in0=modcols[:, 8:16], scalar=1.0, in1=cols[:, l * 24 + C_NW1:l * 24 + C_NW1 + 8],
            op0=ALU.add, op1=ALU.mult))(l), reads=(t_modcols, t_const), writes=(t_wsc,))
        S.emit("dve", (lambda l: I("scalar_tensor_tensor",
            out=wsc[:, 8:16], in0=modcols[:, 24:32], scalar=1.0, in1=cols[:, l * 24 + C_NW2:l * 24 + C_NW2 + 8],
            op0=ALU.add, op1=ALU.mult))(l), reads=(t_modcols, t_const), writes=(t_wsc,))
        S.barrier()
        for s_ in sem_ada:
            S.release_dma_sem(s_)
        restore(s0)

        if not stage_ok():
            break
        sm = snap()
        og_ret = sb([128, 4, T], BF16, "og_ret")
        t_ogret = [Tok("ogret%d" % i) for i in range(NT)]

        s0 = snap()
        wq = sb([128, 8, 1024], BF16, "wq")
        wv = sb([128, 8, 512], BF16, "wv")
        wg = sb([128, 8, 512], BF16, "wg")
        t_wq, t_wv, t_wg = Tok("wq"), Tok("wv"), Tok("wg")
        sems_r = [S.dma_sem() for _ in range(4)]
        load_w(wq[:, :, :], wrqk_d[l], t_wq, sems_r[0])
        load_w(wv[:, :, :], wrv_d[l], t_wv, sems_r[1])
        load_w(wg[:, :, :], wrg_d[l], t_wg, sems_r[2])
        hT = sb([128, 8, 512], BF16, "hT")
        t_hT = [[Tok("hT%d_%d" % (i, c)) for c in range(KC)] for i in range(4)]
        rot = sb([128, 4, 2, 512], F32, "rot")
        t_rot = Tok("rot")
        qk = sb([128, 4, 512], BF16, "qk")
        t_qk = [Tok("qk%d" % i) for i in range(4)]
        t1 = [sb([128, 512], F32, "t1") for _ in range(2)]
        t2 = [sb([128, 512], F32, "t2") for _ in range(2)]
        t_t12 = [Tok("t12a"), Tok("t12b")]
        v_sb = sb([128, 4, 512], BF16, "v_sb")
        t_v = [Tok("v%d" % i) for i in range(4)]
        gate = sb([128, 4, 512], F32, "gate")
        t_gate = Tok("gate")
        kT_sb = [sb([128, 256], BF16, "kT") for _ in range(2)]
        t_kT = [Tok("kT0"), Tok("kT1")]
        msc = [sb([128, 4, 128], BF16, "msc") for _ in range(2)]
        t_msc = [Tok("msc0"), Tok("msc1")]
        R_sb = [sb([128, 2, 128], BF16, "R_sb") for _ in range(3)]
        t_R = [Tok("R0"), Tok("R1"), Tok("R2")]
        sq_bf = [sb([128, 512], BF16, "sq") for _ in range(2)]
        o_sb = [sb([128, 512], F32, "o_sb") for _ in range(2)]
        t_osq = [Tok("osq0"), Tok("osq1")]
        sd = sb([128, 512], F32, "sd")
        rstdo = sb([128, 512], F32, "rstdo")
        yb = sb([128, 512], F32, "y")
        t_tmp = Tok("tmp")
        xn = [sb([128, D], BF16, "xn") for _ in range(2)]
        t_xn = [Tok("xn0"), Tok("xn1")]
        junk = sb([128, D], BF16, "junk")
        t_junk = Tok("junk")
        mmps = [ps(F32, "mm") for _ in range(2)]
        t_mm = [Tok("mm0"), Tok("mm1")]
        tpn = ps(BF16, "tpn")
        t_tpn = Tok("tpn")
        sc_ab = [ps(F32, "sca"), ps(F32, "scb")]
        t_scab = [Tok("sca"), Tok("scb")]
        o_ps = ps(F32, "o")
        t_o = Tok("o")
        R_ps = ps(F32, "Rps")
        t_Rps = Tok("Rps")
        Rst = sb([128, 512], F32, "Rst")
        t_Rst = Tok("Rst")
        S.emit("dve", I("memset", Rst[:, :], 0.0), writes=(t_Rst,))
        nrm_ps = ps(F32, "nrm")
        t_nrm = Tok("nrm")
        tpk = sc_ab[1][:, 256:512].bitcast(BF16)
        t_tpk = t_scab[1]

        load_w(rot[:, :, :, :], rot_d[0], t_rot, sems_r[3])
        for g in range(4):
            norm_stats([g * 4 + i for i in range(4)], junk, t_junk)
            norm_group([g * 4 + i for i in range(4)], hT, t_hT, xn, t_xn, [tpn, o_ps[:, :].bitcast(BF16)], [t_tpn, t_o], 0, 0)
            for qi in range(4):
                m = qi % 2
                isk = qi // 2
                ca = isk * 512 + m * 128
                cb_ = isk * 512 + 256 + m * 128
                for pb, c0 in ((0, ca), (1, cb_)):
                    for c in range(KC):
                        S.emit("pe", (lambda pb, c0, c: I("matmul",
                            mmps[pb][:, :], lhsT=wq[:, c, c0:c0 + 128], rhs=hT[:, c, :],
                            start=(c == 0), stop=(c == KC - 1)))(pb, c0, c),
                               reads=(t_wq, *flat(t_hT)), writes=(t_mm[pb],))
                tb = qi % 2
                S.emit("dve", (lambda tb, isk, m: I("tensor_tensor",
                    out=t1[tb][:, :], in0=mmps[0][:, :], in1=rot[:, 2 * isk, m, :], op=ALU.mult))(tb, isk, m),
                       reads=(t_mm[0], t_rot), writes=(t_t12[tb],))
                S.emit("dve", (lambda tb, isk, m: I("tensor_tensor",
                    out=t2[tb][:, :], in0=mmps[1][:, :], in1=rot[:, 2 * isk + 1, m, :], op=ALU.mult))(tb, isk, m),
                       reads=(t_mm[1], t_rot), writes=(t_t12[tb],))
                S.emit("dve", (lambda tb, qi: I("tensor_tensor",
                    out=qk[:, qi, :], in0=t1[tb][:, :], in1=t2[tb][:, :], op=ALU.add))(tb, qi),
                       reads=(t_t12[tb],), writes=(t_qk[qi],))
            if g + 1 < 4:
                load_w(rot[:, :, :, :], rot_d[g + 1], t_rot, sems_r[3])
            for i in range(4):
                pb = i % 2
                for c in range(KC):
                    S.emit("pe", (lambda pb, i, c: I("matmul",
                        mmps[pb][:, :], lhsT=hT[:, c, i * 128:(i + 1) * 128], rhs=wv[:, c, :],
                        start=(c == 0), stop=(c == KC - 1)))(pb, i, c),
                           reads=(t_wv, *t_hT[i]), writes=(t_mm[pb],))
                S.emit("act", (lambda pb, i: I("activation", out=v_sb[:, i, :], in_=mmps[pb][:, :],
                                                                    func=AF.Copy))(pb, i),
                       reads=(t_mm[pb],), writes=(t_v[i],))
            for m in range(4):
                pb = m % 2
                for c in range(KC):
                    S.emit("pe", (lambda pb, m, c: I("matmul",
                        mmps[pb][:, :], lhsT=wg[:, c, m * 128:(m + 1) * 128], rhs=hT[:, c, :],
                        start=(c == 0), stop=(c == KC - 1)))(pb, m, c),
                           reads=(t_wg, *flat(t_hT)), writes=(t_mm[pb],))
                S.emit("act", (lambda pb, m: I("activation", out=gate[:, m, :], in_=mmps[pb][:, :],
                                                                    func=AF.Silu))(pb, m),
                       reads=(t_mm[pb],), writes=(t_gate,))

            def stA(i, g=g):
                b = i % 2
                gi = g * 4 + i
                cs = slice(i * 128, (i + 1) * 128)
                for m in range(2):
                    S.emit("pe", (lambda m: I("transpose", out=tpk[:, m * 128:(m + 1) * 128],
                                                                  in_=qk[:, 2 + m, cs], identity=ident[:, :]))(m),
                           reads=(t_qk[2 + m], t_ident), writes=(t_tpk,))
                S.emit("act", I("activation", out=kT_sb[b][:, :], in_=tpk[:, 0:256], func=AF.Copy),
                       reads=(t_tpk,), writes=(t_kT[b],))
                for h in range(4):
                    p0 = (h % 2) * 64
                    S.emit("pe", (lambda h, p0: I("matmul",
                        sc_ab[h % 2][:, (h // 2) * 128:(h // 2 + 1) * 128], lhsT=qk[p0:p0 + 64, 2 + h // 2, cs],
                        rhs=qk[p0:p0 + 64, h // 2, cs], start=True, stop=True))(h, p0),
                           reads=(t_qk[2 + h // 2], t_qk[h // 2]), writes=(t_scab[h % 2],))
                for par in range(2):
                    S.emit("dve", (lambda par: I("tensor_tensor",
                        out=msc[b][:, 2 * par:2 * par + 2, :],
                        in0=sc_ab[par][:, 0:256].rearrange("p (h t) -> p h t", h=2),
                        in1=mask_ret.unsqueeze(1).broadcast_to([128, 2, 128]), op=ALU.mult))(par),
                           reads=(t_scab[par], t_const), writes=(t_msc[b],))

            def stA2(i, g=g):
                b = i % 2
                gi = g * 4 + i
                if gi < NT - 1:
                    for hp in range(2):
                        S.emit("pe", (lambda hp: I("matmul",
                            R_ps[:, hp * 256:(hp + 1) * 256], lhsT=kT_sb[b][:, hp * 128:(hp + 1) * 128],
                            rhs=v_sb[:, i, hp * 256:(hp + 1) * 256], start=(hp == 0), stop=(hp == 1)))(hp),
                               reads=(t_kT[b], t_v[i]), writes=(t_Rps,))
                    S.emit("dve", I("tensor_tensor", out=Rst[:, :], in0=R_ps[:, :], in1=Rst[:, :], op=ALU.add),
                           reads=(t_Rps, t_Rst), writes=(t_Rst,))
                    for hl in range(2):
                        S.emit("act", I("activation", out=R_sb[(gi + 1) % 3][hl * 64:(hl + 1) * 64, :, :],
                                        in_=Rst[hl * 64:(hl + 1) * 64, :].rearrange(
                                            "p (hp x) -> p hp x", hp=2)[:, :, hl * 128:(hl + 1) * 128],
                                        func=AF.Copy),
                               reads=(t_Rst,), writes=(t_R[(gi + 1) % 3],))

            def stB(i, g=g):
                b = i % 2
                gi = g * 4 + i
                cs = slice(i * 128, (i + 1) * 128)
                for h in range(4):
                    p0 = (h % 2) * 64
                    S.emit("pe", (lambda h: I("matmul",
                        o_ps[:, h * 128:(h + 1) * 128], lhsT=v_sb[:, i, h * 128:(h + 1) * 128],
                        rhs=msc[b][:, (h % 2) * 2 + h // 2, :], start=True, stop=(gi == 0)))(h),
                           reads=(t_v[i], t_msc[b]), writes=(t_o,))
                    if gi > 0:
                        S.emit("pe", (lambda h, p0: I("matmul",
                            o_ps[:, h * 128:(h + 1) * 128], lhsT=R_sb[gi % 3][p0:p0 + 64, h // 2, :],
                            rhs=qk[p0:p0 + 64, h // 2, cs], start=False, stop=True))(h, p0),
                               reads=(t_R[gi % 3], t_qk[h // 2]), writes=(t_o,))
                S.emit("act", I("activation", out=sq_bf[b][:, :], in_=o_ps[:, :], func=AF.Square),
                       reads=(t_o,), writes=(t_osq[b],))
                S.emit("act", I("activation", out=o_sb[b][:, :], in_=o_ps[:, :], func=AF.Copy),
                       reads=(t_o,), writes=(t_osq[b],))

            def stC(i, g=g, l=l):
                b = i % 2
                gi = g * 4 + i
                head_norm_gate(o_sb[b], sq_bf[b], t_osq[b], nrm_ps, t_nrm, sd, rstdo, yb, t_tmp, gate, i * 128,
                               t_gate, l * 24 + C_RNW,
                               og_ret[:, :, gi * 128:(gi + 1) * 128], t_ogret[gi])

            for step in range(4 + 2):
                if step < 4:
                    stA(step)
                if 0 <= step - 2 < 4:
                    stC(step - 2)
                if 0 <= step - 1 < 4:
                    stB(step - 1)
                if step < 4:
                    stA2(step)
        S.barrier()
        for s_ in sems_r:
            S.release_dma_sem(s_)
        restore(s0)

        if not stage_ok():
            restore(sm)
            break
        s0 = snap()
        whq = sb([128, 8, 512], BF16, "whq")
        whf = sb([128, 8, 512], BF16, "whf")
        whi = sb([128, 8, 512], BF16, "whi")
        whg = sb([128, 8, 512], BF16, "whg")
        wo = sb([128, 8, 1024], BF16, "wo")
        t_whq, t_whf, t_whi, t_whg, t_wo = Tok("whq"), Tok("whf"), Tok("whi"), Tok("whg"), Tok("wo")
        sems_h = [S.dma_sem() for _ in range(5)]
        load_w(whi[:, :, :], whi_d[l], t_whi, sems_h[2])
        load_w(whg[:, :, :], whg_d[l], t_whg, sems_h[3])
        load_w(whf[:, :, :], whf_d[l], t_whf, sems_h[0])
        load_w(whq[:, :, :], whq_d[l], t_whq, sems_h[1])
        load_w(wo[:, :, :], wo_d[l], t_wo, sems_h[4])
        hT = sb([128, 8, 512], BF16, "hT")
        t_hT = [[Tok("hT%d_%d" % (i, c)) for c in range(KC)] for i in range(4)]
        B1 = sb([128, 512], F32, "B1")
        B2 = sb([128, 512], F32, "B2")
        B3 = sb([128, 512], F32, "B3")
        B4 = sb([128, 512], F32, "B4")
        t_B1, t_B2, t_B3, t_B4 = Tok("B1"), Tok("B2"), Tok("B3"), Tok("B4")
        Aend = sb([128, 4, 8], F32, "Aend")
        t_A = Tok("Aend")
        qfb = sb([128, 4, 512], BF16, "qfb")
        t_qf = [Tok("qf%d" % i) for i in range(4)]
        qt = sb([128, 4, 512], BF16, "qt")
        t_qt = Tok("qt")
        kt = sb([128, 4, 512], BF16, "kt")
        t_kt = Tok("kt")
        vh = sb([128, 4, 512], BF16, "vh")
        t_vh = [Tok("vh%d" % i) for i in range(4)]
        gate = sb([128, 4, 512], BF16, "gateh")
        t_gate = Tok("gateh")
        kT_sb = [sb([128, 4, 128], BF16, "kTh") for _ in range(2)]
        t_kT = [Tok("kTh0"), Tok("kTh1")]
        msc = [sb([128, 4, 128], BF16, "msch") for _ in range(2)]
        t_msc = [Tok("msch0"), Tok("msch1")]
        Sst = sb([128, 512], F32, "Sst")
        Stmp = sb([128, 512], F32, "Stmp")
        S_ring = [sb([128, 4, 128], BF16, "S_ring") for _ in range(5)]
        t_S = Tok("S")
        t_Sr = [Tok("Sr%d" % i) for i in range(5)]
        sq_bf = [sb([128, 512], BF16, "sqh") for _ in range(2)]
        o_sb = [sb([128, 512], F32, "o_sbh") for _ in range(2)]
        t_osq = [Tok("osqh0"), Tok("osqh1")]
        sd = sb([128, 512], F32, "sdh")
        t_tmp = Tok("tmph")
        og_hg = [sb([128, 4, 128], BF16, "og_hg") for _ in range(2)]
        t_oghg = [Tok("oghg0"), Tok("oghg1")]
        xn = [sb([128, D], BF16, "xnh")] * 2
        t_xn = [Tok("xnh0")] * 2
        mmps = [ps(F32, "mmh") for _ in range(2)]
        t_mm = [Tok("mmh0"), Tok("mmh1")]
        tpn = ps(BF16, "tpnh")
        t_tpn = Tok("tpnh")
        tpk = ps(BF16, "tpkh")
        t_tpk = Tok("tpkh")
        sc_ps = ps(F32, "sch")
        t_sc = Tok("sch")
        o_ps = ps(F32, "oh")
        t_o = Tok("oh")
        P_ps = ps(F32, "Pps")
        t_P = Tok("Pps")
        nrm_ps = ps(F32, "nrmh")
        t_nrm = Tok("nrmh")

        def scale_wo(l=l):
            for kc in range(8):
                nwc = l * 24 + (C_RNW + kc if kc < 4 else C_HNW + kc - 4)
                S.emit("dve", I("scalar_tensor_tensor", out=wo[:, kc, :], in0=wo[:, kc, :],
                                scalar=cols[:, nwc:nwc + 1], in1=grow[:, 0, :], op0=ALU.mult, op1=ALU.mult),
                       reads=(t_wo, t_grow[0], t_const), writes=(t_wo,))
        S.emit("dve", I("memset", Sst[:, :], 0.0), writes=(t_S,))
        S.emit("dve", I("memset", S_ring[0][:, :, :], 0.0), writes=(t_Sr[0],))
        lo = l * 12
        for g in range(4):
            norm_group([g * 4 + i for i in range(4)], hT, t_hT, xn, t_xn, [tpn, o_ps[:, :].bitcast(BF16)], [t_tpn, t_o], 0, 0)
            for i in range(4):
                pb = i % 2
                for c in range(KC):
                    S.emit("pe", (lambda pb, i, c: I("matmul",
                        mmps[pb][:, :], lhsT=hT[:, c, i * 128:(i + 1) * 128], rhs=whi[:, c, :],
                        start=(c == 0), stop=(c == KC - 1)))(pb, i, c),
                           reads=(t_whi, *t_hT[i]), writes=(t_mm[pb],))
                S.emit("act", (lambda pb, i: I("activation", out=vh[:, i, :], in_=mmps[pb][:, :],
                                                                    func=AF.Copy))(pb, i),
                       reads=(t_mm[pb],), writes=(t_vh[i],))
            for m in range(4):
                pb = m % 2
                for c in range(KC):
                    S.emit("pe", (lambda pb, m, c: I("matmul",
                        mmps[pb][:, :], lhsT=whg[:, c, m * 128:(m + 1) * 128], rhs=hT[:, c, :],
                        start=(c == 0), stop=(c == KC - 1)))(pb, m, c),
                           reads=(t_whg, *flat(t_hT)), writes=(t_mm[pb],))
                S.emit("act", (lambda pb, m: I("activation", out=gate[:, m, :], in_=mmps[pb][:, :],
                                                                    func=AF.Silu))(pb, m),
                       reads=(t_mm[pb],), writes=(t_gate,))
            for m in range(4):
                pb = m % 2
                for c in range(KC):
                    S.emit("pe", I("matmul", mmps[pb][:, :], lhsT=whq[:, c, m * 128:(m + 1) * 128], rhs=hT[:, c, :],
                                   start=(c == 0), stop=(c == KC - 1)),
                           reads=(t_whq, *flat(t_hT)), writes=(t_mm[pb],))
                S.emit("act", I("activation", out=qfb[:, m, :], in_=mmps[pb][:, :], func=AF.Silu),
                       reads=(t_mm[pb],), writes=(t_qf[m],))
            for m in range(4):
                pb = m % 2
                for c in range(KC):
                    S.emit("pe", I("matmul", mmps[pb][:, :], lhsT=whf[:, c, m * 128:(m + 1) * 128], rhs=hT[:, c, :],
                                   start=(c == 0), stop=(c == KC - 1)),
                           reads=(t_whf, *flat(t_hT)), writes=(t_mm[pb],))
                S.emit("act", I("activation", out=B1[:, :], in_=mmps[pb][:, :], func=AF.Exp, scale=-1.0),
                       reads=(t_mm[pb],), writes=(t_B1,))
                S.emit("act", I("activation", out=B1[:, :], in_=B1[:, :], func=AF.Ln, bias=ones_f[:, 0:1]),
                       reads=(t_B1, t_const), writes=(t_B1,))
                S.emit("act", I("activation", out=B2[:, :], in_=B1[:, :], func=AF.Exp, scale=-1.0),
                       reads=(t_B1,), writes=(t_B2,))
                S.emit("act", I("activation", out=B1[:, :], in_=B2[:, :], func=AF.Ln,
                                scale=lbc[:, lo + m:lo + m + 1], bias=lbc[:, lo + 4 + m:lo + 4 + m + 1]),
                       reads=(t_B2, t_lbc), writes=(t_B1,))
                S.emit("dve", I("tensor_scalar", out=B2[:, :], in0=B2[:, :], scalar1=lbc[:, lo + 8 + m:lo + 8 + m + 1],
                                scalar2=lbc[:, lo + m:lo + m + 1], op0=ALU.mult, op1=ALU.add),
                       reads=(t_B2, t_lbc), writes=(t_B2,))
                S.emit("dve", I("tensor_tensor_scan", out=B3[:, :], data0=resetmask, data1=B1[:, :], initial=0.0,
                                op0=ALU.mult, op1=ALU.add), reads=(t_B1, t_const), writes=(t_B3,))
                S.emit("dve", I("tensor_scalar", out=B3[:, :], in0=B3[:, :], scalar1=-80.0, scalar2=None,
                                op0=ALU.max), reads=(t_B3,), writes=(t_B3,))
                S.emit("act", I("activation", out=B4[:, :], in_=B3[:, :], func=AF.Exp),
                       reads=(t_B3,), writes=(t_B4,))
                S.emit("act", I("activation", out=B1[:, :], in_=B3[:, :], func=AF.Exp, scale=-1.0),
                       reads=(t_B3,), writes=(t_B1,))
                S.emit("dve", I("tensor_tensor", out=kt[:, m, :], in0=B2[:, :], in1=B1[:, :], op=ALU.mult),
                       reads=(t_B2, t_B1), writes=(t_kt,))
                S.emit("dve", I("tensor_copy", out=Aend[:, m, :],
                                in_=B4[:, :].rearrange("p (c t) -> p c t", t=64)[:, :, 63]),
                       reads=(t_B4,), writes=(t_A,))
                S.emit("dve", I("tensor_tensor", out=qt[:, m, :], in0=qfb[:, m, :], in1=B4[:, :], op=ALU.mult),
                       reads=(t_qf[m], t_B4), writes=(t_qt,))

            def stA(i, g=g):
                b = i % 2
                cs = slice(i * 128, (i + 1) * 128)
                for h in range(4):
                    S.emit("pe", (lambda h: I("transpose", out=tpk[:, h * 128:(h + 1) * 128],
                                                                  in_=kt[:, h, cs], identity=ident[:, :]))(h),
                           reads=(t_kt, t_ident), writes=(t_tpk,))
                S.emit("act", I("activation", out=kT_sb[b][:, :, :],
                                                     in_=tpk[:, 0:512].rearrange("p (h t) -> p h t", h=4),
                                                     func=AF.Copy),
                       reads=(t_tpk,), writes=(t_kT[b],))
                for h in range(4):
                    S.emit("pe", (lambda h: I("matmul",
                        sc_ps[:, h * 128:(h + 1) * 128], lhsT=kt[:, h, cs], rhs=qt[:, h, cs],
                        start=True, stop=True))(h),
                           reads=(t_kt, t_qt), writes=(t_sc,))
                S.emit("dve", I("tensor_tensor",
                    out=msc[b][:, :, :], in0=sc_ps[:, :].rearrange("p (h t) -> p h t", h=4),
                    in1=mask_hg.unsqueeze(1).broadcast_to([128, 4, 128]), op=ALU.mult),
                       reads=(t_sc, t_const), writes=(t_msc[b],))

            def stA2(i, g=g):
                b = i % 2
                gi = g * 4 + i
                for j in range(2):
                    r0 = j * 64
                    n = 2 * gi + j
                    for h in range(4):
                        S.emit("pe", (lambda h: I("matmul",
                            P_ps[:, h * 128:(h + 1) * 128], lhsT=kT_sb[b][r0:r0 + 64, h, :],
                            rhs=vh[r0:r0 + 64, i, h * 128:(h + 1) * 128], start=True, stop=True))(h),
                               reads=(t_kT[b], t_vh[i]), writes=(t_P,))
                    S.emit("dve", I("tensor_tensor", out=Stmp[:, :], in0=P_ps[:, :], in1=Sst[:, :], op=ALU.add),
                           reads=(t_P, t_S), writes=(t_S,))
                    S.emit("dve", I("tensor_tensor",
                        out=Sst[:, :].rearrange("p (h v) -> p h v", h=4),
                        in0=Stmp[:, :].rearrange("p (h v) -> p h v", h=4),
                        in1=Aend[:, :, i * 2 + j:i * 2 + j + 1].broadcast_to([128, 4, 128]), op=ALU.mult),
                           reads=(t_S, t_A), writes=(t_S,))
                    S.emit("act", I("activation", out=S_ring[(n + 1) % 5][:, :, :],
                                    in_=Sst[:, :].rearrange("p (h v) -> p h v", h=4), func=AF.Copy),
                           reads=(t_S,), writes=(t_Sr[(n + 1) % 5],))

            def stB(i, g=g):
                b = i % 2
                gi = g * 4 + i
                for h in range(4):
                    S.emit("pe", (lambda h: I("matmul",
                        o_ps[:, h * 128:(h + 1) * 128], lhsT=vh[:, i, h * 128:(h + 1) * 128],
                        rhs=msc[b][:, h, :], start=(h == 0), stop=False))(h),
                           reads=(t_vh[i], t_msc[b]), writes=(t_o,))
                for j in range(2):
                    c0 = i * 128 + j * 64
                    r0 = j * 64
                    n = 2 * gi + j
                    for h in range(4):
                        S.emit("pe", (lambda h: I("matmul",
                            o_ps[:, h * 128 + r0:h * 128 + r0 + 64], lhsT=S_ring[n % 5][:, h, :],
                            rhs=qt[:, h, c0:c0 + 64], start=False, stop=(j == 1 and h == 3)))(h),
                               reads=(t_Sr[n % 5], t_qt), writes=(t_o,))
                S.emit("act", I("activation", out=sq_bf[b][:, :], in_=o_ps[:, :], func=AF.Square),
                       reads=(t_o,), writes=(t_osq[b],))
                S.emit("act", I("activation", out=o_sb[b][:, :], in_=o_ps[:, :], func=AF.Copy),
                       reads=(t_o,), writes=(t_osq[b],))

            def stC(i, g=g, l=l):
                b = i % 2
                gi = g * 4 + i
                head_norm_gate(o_sb[b], sq_bf[b], t_osq[b], nrm_ps, t_nrm, sd, sd, o_sb[b], t_tmp, gate, i * 128,
                               t_gate, l * 24 + C_HNW, og_hg[b][:, :, :], t_oghg[b])

            def stC2(i, g=g):
                b = i % 2
                gi = g * 4 + i
                for nh in range(2):
                    pb = nh
                    for kc in range(8):
                        if kc < 4:
                            lhs = og_ret[:, kc, gi * 128:(gi + 1) * 128]
                            rd = t_ogret[gi]
                        else:
                            lhs = og_hg[b][:, kc - 4, :]
                            rd = t_oghg[b]
                        S.emit("pe", I("matmul", mmps[pb][:, :], lhsT=lhs, rhs=wo[:, kc, nh * 512:(nh + 1) * 512],
                                       start=(kc == 0), stop=(kc == 7)),
                               reads=(rd, t_wo), writes=(t_mm[pb],))
                    S.emit("dve", I("tensor_tensor", out=x_sb[:, gi, nh * 512:(nh + 1) * 512],
                                    in0=mmps[pb][:, :], in1=x_sb[:, gi, nh * 512:(nh + 1) * 512], op=ALU.add),
                           reads=(t_mm[pb], x_tok[gi]), writes=(x_tok[gi],))

            if g == 0:
                scale_wo()
            for step in range(4 + 3):
                if step < 4:
                    stA(step)
                if 0 <= step - 2 < 4:
                    stC(step - 2)
                if 0 <= step - 3 < 4:
                    stC2(step - 3)
                if 0 <= step - 1 < 4:
                    stB(step - 1)
                if step < 4:
                    stA2(step)
        S.barrier()
        for s_ in sems_h:
            S.release_dma_sem(s_)
        restore(s0)
        restore(sm)

        if not stage_ok():
            break
        s0 = snap()
        hT2 = sb([128, 8, T], BF16, "hT2")
        t_hT2 = [[Tok("hT2_%d_%d" % (i, c)) for c in range(KC)] for i in range(NT)]
        a_sb = sb([128, 11, 1024], BF16, "a")
        t_a = [Tok("a%d" % j) for j in range(11)]
        NWS = 3
        wgu = [sb([128, 2, 8, 128], BF16, "wgu") for _ in range(NWS)]
        t_wgu = [Tok("wgu%d" % i) for i in range(NWS)]
        sem_wgu = [S.dma_sem() for _ in range(NWS)]
        NWD = 3
        wdb = [sb([128, 11, 512], BF16, "wd") for _ in range(NWD)]
        t_wd = [Tok("wd%d" % i) for i in range(NWD)]
        sem_wd = [S.dma_sem() for _ in range(NWD)]
        sg = [sb([128, 512], F32, "sg") for _ in range(2)]
        t_sg = [Tok("sg0"), Tok("sg1")]
        rtmp = [sb([128, 512], F32, "rtmpf") for _ in range(2)]
        t_rtmp = [Tok("rtmpf0"), Tok("rtmpf1")]
        xn = [sb([128, D], BF16, "xnf") for _ in range(2)]
        t_xn = [Tok("xnf0"), Tok("xnf1")]
        junk = sb([128, D], BF16, "junkf")
        t_junk = Tok("junkf")
        gps = [ps(F32, "gps") for _ in range(2)]
        ups = [ps(F32, "ups") for _ in range(2)]
        t_gps = [Tok("gps0"), Tok("gps1")]
        t_ups = [Tok("ups0"), Tok("ups1")]
        tpn = ps(BF16, "tpnf")
        t_tpn = Tok("tpnf")
        tpn2 = ps(BF16, "tpnf2")
        t_tpn2 = Tok("tpnf2")
        dps = [ps(F32, "dps") for _ in range(2)]
        t_dps = [Tok("dps0"), Tok("dps1")]

        slab_seq = [j for _th in range(2) for j in range(NJ)]
        wd_seq = [(ffh, nh) for _th in range(2) for ffh in range(2) for nh in range(2)]

        def load_slab(q, l=l):
            j = slab_seq[q]
            slot = q % NWS
            S.emit("pool", I("dma_start", out=wgu[slot][:, 0, :, :], in_=wgs_d[l, j]),
                   writes=(t_wgu[slot],), dsem=sem_wgu[slot])
            S.emit("pool", I("dma_start", out=wgu[slot][:, 1, :, :], in_=wus_d[l, j]),
                   writes=(t_wgu[slot],), dsem=sem_wgu[slot])

        def load_wd(q, l=l):
            ffh, nh = wd_seq[q]
            slot = q % NWD
            S.emit("pool", I("dma_start", out=wdb[slot][:, :, :],
                             in_=wd_d[l, :, ffh * 11:(ffh + 1) * 11, nh * 512:(nh + 1) * 512]),
                   writes=(t_wd[slot],), dsem=sem_wd[slot])

        for q in range(NWS):
            load_slab(q)
        for q in range(NWD):
            load_wd(q)

        def ffn_norm(th):
            tiles = list(range(th * 8, th * 8 + 8))
            norm_stats(tiles, junk, t_junk)
            norm_group(tiles, hT2[:, :, th * 1024:(th + 1) * 1024], t_hT2[th * 8:(th + 1) * 8],
                       xn, t_xn, [tpn, tpn2], [t_tpn, t_tpn2], 8, 16)

        ffn_norm(0)
        ev_ctr = 0
        sq = 0
        wq_i = 0
        for th in range(2):
            tiles = list(range(th * 8, th * 8 + 8))
            for ffh in range(2):
                for jj in range(11):
                    slot = sq % NWS
                    for g2 in range(2):
                        pb = (sq * 2 + g2) % 2
                        c0 = th * 1024 + g2 * 512
                        rd_h = flat(t_hT2[th * 8 + g2 * 4:th * 8 + g2 * 4 + 4])
                        for c in range(KC):
                            S.emit("pe", I("matmul", gps[pb][:, :], lhsT=wgu[slot][:, 0, c, :],
                                           rhs=hT2[:, c, c0:c0 + 512], start=(c == 0), stop=(c == KC - 1)),
                                   reads=(t_wgu[slot], *rd_h), writes=(t_gps[pb],))
                        for c in range(KC):
                            S.emit("pe", I("matmul", ups[pb][:, :], lhsT=wgu[slot][:, 1, c, :],
                                           rhs=hT2[:, c, c0:c0 + 512], start=(c == 0), stop=(c == KC - 1)),
                                   reads=(t_wgu[slot], *rd_h), writes=(t_ups[pb],))
                        S.emit("act", I("activation", out=sg[pb][:, :], in_=gps[pb][:, :], func=AF.Silu),
                               reads=(t_gps[pb],), writes=(t_sg[pb],))
                        S.emit("dve", I("tensor_tensor", out=a_sb[:, jj, g2 * 512:(g2 + 1) * 512],
                                        in0=sg[pb][:, :], in1=ups[pb][:, :], op=ALU.mult),
                               reads=(t_sg[pb], t_ups[pb]), writes=(t_a[jj],))
                    if sq + NWS < len(slab_seq):
                        load_slab(sq + NWS)
                    sq += 1
                if th == 0 and ffh == 0:
                    ffn_norm(1)
                for nh in range(2):
                    slot = wq_i % NWD
                    for ii, gi in enumerate(tiles):
                        pb = ev_ctr % 2
                        ev_ctr += 1
                        for jj in range(11):
                            S.emit("pe", I("matmul", dps[pb][:, :], lhsT=a_sb[:, jj, ii * 128:(ii + 1) * 128],
                                           rhs=wdb[slot][:, jj, :], start=(jj == 0), stop=(jj == 10)),
                                   reads=(t_a[jj], t_wd[slot]), writes=(t_dps[pb],))
                        S.emit("dve", I("tensor_tensor", out=rtmp[pb][:, :], in0=dps[pb][:, :],
                                        in1=grow[:, 1, nh * 512:(nh + 1) * 512], op=ALU.mult),
                               reads=(t_dps[pb], t_grow[1]), writes=(t_rtmp[pb],))
                        S.emit("dve", I("tensor_tensor", out=x_sb[:, gi, nh * 512:(nh + 1) * 512],
                                        in0=x_sb[:, gi, nh * 512:(nh + 1) * 512], in1=rtmp[pb][:, :], op=ALU.add),
                               reads=(t_rtmp[pb], x_tok[gi]), writes=(x_tok[gi],))
                    if wq_i + NWD < len(wd_seq):
                        load_wd(wq_i + NWD)
                    wq_i += 1
        S.barrier()
        for s_ in sem_wgu + sem_wd:
            S.release_dma_sem(s_)
        restore(s0)

    final = stage[0] <= n_stage and n_stage >= 99
    s0 = snap()
    fw = sb([128, D], F32, "fw")
    t_fw = Tok("fw")
    sem_fw = S.dma_sem("sp")
    S.emit("sp", I("dma_start", out=fw[:, :], in_=fw_d[:, :]), writes=(t_fw,), dsem=sem_fw)
    obuf = [sb([128, D], F32, "obuf") for _ in range(2)]
    t_ob = [Tok("ob0"), Tok("ob1")]
    sem_out = [S.dma_sem("sp"), S.dma_sem("sp")]
    junk = sb([128, D], BF16, "junko")
    t_junk = Tok("junko")
    if final:
        norm_stats(list(range(NT)), junk, t_junk)
    for i in range(NT):
        b = i % 2
        if final:
            S.emit("dve", (lambda i, b: I("scalar_tensor_tensor",
                out=obuf[b][:, :], in0=x_sb[:, i, :], scalar=rstdx[:, i:i + 1], in1=fw[:, :],
                op0=ALU.mult, op1=ALU.mult))(i, b), reads=(x_tok[i], t_rstdx, t_fw), writes=(t_ob[b],))
            S.emit("sp", (lambda i, b: I("dma_start", out=out_d[i * 128:(i + 1) * 128, :],
                                                             in_=obuf[b][:, :]))(i, b),
                   reads=(t_ob[b],), dsem=sem_out[b])
        else:
            S.emit("sp", (lambda i: I("dma_start", out=out_d[i * 128:(i + 1) * 128, :],
                                                          in_=x_sb[:, i, :]))(i),
                   reads=(x_tok[i],), dsem=sem_out[b])
    for b in range(2):
        t = Tok()
        t.lw = Ev(sem_out[b], sem_out[b].count, {})
        S.emit("sp", None, reads=(t,))
    restore(s0)

    with nc.Block() as block:
        @block.tensor
        def _(e):
            S.replay("pe", e)

        @block.scalar
        def _(e):
            S.replay("act", e)

        @block.vector
        def _(e):
            S.replay("dve", e)

        @block.gpsimd
        def _(e):
            S.replay("pool", e)

        @block.sync
        def _(e):
            S.replay("sp", e)
    return nc


def _rot_tables():
    H, DK = 4, 64
    inv = (10000.0 ** (-np.linspace(0.0, 1.0, DK // 2, dtype=np.float32))).astype(np.float32)
    pos = np.arange(T, dtype=np.float32)
    theta = (pos[:, None] * inv[None, :]).astype(np.float32).astype(np.float64)
    cos = np.cos(theta)
    sin = np.sin(theta)
    lg = np.log(1.0 - np.power(2.0, -5.0 - np.arange(H, dtype=np.float64)))
    tt = np.arange(T, dtype=np.float64)
    tabs = np.zeros((4, 128, 2, T), dtype=np.float64)
    for m in range(2):
        for p in range(128):
            h = 2 * m + p // 64
            dd = p % 64
            i = dd // 2
            sgn = -1.0 if dd % 2 == 0 else 1.0
            dq = np.exp(lg[h] * tt)
            dk = np.exp(-lg[h] * tt) * (DK ** -0.5)
            tabs[0, p, m] = cos[:, i] * dq
            tabs[1, p, m] = sgn * sin[:, i] * dq
            tabs[2, p, m] = cos[:, i] * dk
            tabs[3, p, m] = sgn * sin[:, i] * dk
    tabs = tabs.reshape(4, 128, 2, 4, 512).transpose(3, 1, 0, 2, 4)
    return np.ascontiguousarray(tabs.astype(np.float32))


def _consts():
    c = np.zeros((128, 128 * 3 + 512), dtype=np.float32)
    c[:, 0:128] = np.eye(128, dtype=np.float32)
    s = np.arange(128)[:, None]
    t = np.arange(128)[None, :]
    c[:, 128:256] = (s <= t).astype(np.float32)
    c[:, 256:384] = ((s <= t) & (s // 64 == t // 64)).astype(np.float32)
    rm = np.ones(512, dtype=np.float32)
    rm[0::64] = 0.0
    c[:, 384:896] = rm[None, :]
    return c


def _pcn(w):
    K, N = w.shape
    return np.ascontiguousarray(w.reshape(K // 128, 128, N).transpose(1, 0, 2))


_NC_CACHE = {}


def _prep_shared(w_ada, b_ada, w_in, w_out, w_ffn_gate, w_ffn_up, w_ffn_down, final_norm_w):
    f = np.float32
    sh = {}
    ada = np.zeros((L, 1152, 6 * D), dtype=f)
    ada[:, :D] = w_ada
    ada[:, D] = b_ada
    sh["w_ada"] = np.ascontiguousarray(ada.reshape(L, 9, 128, 12, 512).transpose(0, 3, 2, 1, 4))
    swap = np.arange(256) ^ 1
    rq = w_in[:, :, 0:256]
    rk = w_in[:, :, 256:512]
    rqk = np.concatenate([rq, rq[:, :, swap], rk, rk[:, :, swap]], axis=2)
    sh["w_rqk"] = np.stack([_pcn(rqk[l]) for l in range(L)])
    names = [("w_rv", 512), ("w_rg", 1024), ("w_hq", 1536), ("w_hf", 2048), ("w_hi", 2560), ("w_hg", 3072)]
    for nm, o in names:
        sh[nm] = np.stack([_pcn(w_in[l][:, o:o + 512]) for l in range(L)])
    sh["w_o"] = np.stack([_pcn(w_out[l]) for l in range(L)])
    sh["w_gs"] = np.ascontiguousarray(w_ffn_gate.reshape(L, 8, 128, NJ, 128).transpose(0, 3, 2, 1, 4))
    sh["w_us"] = np.ascontiguousarray(w_ffn_up.reshape(L, 8, 128, NJ, 128).transpose(0, 3, 2, 1, 4))
    sh["w_d"] = np.ascontiguousarray(w_ffn_down.reshape(L, NJ, 128, D).transpose(0, 2, 1, 3))
    sh["fwrow"] = np.ascontiguousarray(np.broadcast_to(final_norm_w[None, :], (128, D))).astype(f)
    sh["rot"] = _rot_tables()
    sh["consts"] = _consts()
    return sh


def _cols_for(b, c, norm_mix_w, norm_ffn_w, ret_norm_w, hg_norm_w, hg_lower_bounds):
    cols = np.zeros((128, NCOLS), dtype=np.float32)
    for l in range(L):
        cols[:, l * 24 + C_NW1:l * 24 + C_NW1 + 8] = norm_mix_w[l].reshape(8, 128).T
        cols[:, l * 24 + C_NW2:l * 24 + C_NW2 + 8] = norm_ffn_w[l].reshape(8, 128).T
        cols[:, l * 24 + C_RNW:l * 24 + C_RNW + 4] = ret_norm_w[l].reshape(4, 128).T
        cols[:, l * 24 + C_HNW:l * 24 + C_HNW + 4] = hg_norm_w[l].reshape(4, 128).T
    cols[:, C_LB:C_LB + 4] = hg_lower_bounds[0].reshape(4, 128).T
    cols[:, C_LB + 4:C_LB + 8] = hg_lower_bounds[1].reshape(4, 128).T
    cols[:, C_C:C_C + 8] = c[b].reshape(8, 128).T
    return cols


def kernel(x, c, w_ada, b_ada, norm_mix_w, w_in, ret_norm_w, hg_lower_bounds, hg_norm_w, w_out,
           norm_ffn_w, w_ffn_gate, w_ffn_up, w_ffn_down, final_norm_w, _n_stage=99):
    arrs = [np.asarray(a, dtype=np.float32) for a in
            (x, c, w_ada, b_ada, norm_mix_w, w_in, ret_norm_w, hg_lower_bounds, hg_norm_w, w_out,
             norm_ffn_w, w_ffn_gate, w_ffn_up, w_ffn_down, final_norm_w)]
    (x, c, w_ada, b_ada, norm_mix_w, w_in, ret_norm_w, hg_lower_bounds, hg_norm_w, w_out,
     norm_ffn_w, w_ffn_gate, w_ffn_up, w_ffn_down, final_norm_w) = arrs
    shared = _prep_shared(w_ada, b_ada, w_in, w_out, w_ffn_gate, w_ffn_up, w_ffn_down, final_norm_w)
    if _n_stage not in _NC_CACHE:
        _NC_CACHE[_n_stage] = build_nc(_n_stage)
    nc = _NC_CACHE[_n_stage]
    in_maps = []
    for b in range(8):
        m = dict(shared)
        m["x"] = np.ascontiguousarray(x[b])
        m["cols"] = _cols_for(b, c, norm_mix_w, norm_ffn_w, ret_norm_w, hg_norm_w, hg_lower_bounds)
        in_maps.append(m)
    res = run_bass_kernel_spmd(nc, in_maps, core_ids=list(range(8)))
    return np.stack([np.asarray(r["out"], dtype=np.float32).reshape(T, D) for r in res.results], axis=0)
```
